# Optimizing a Trainium2 kernel written in Bass

```python
import math
import jax, jax.numpy as jnp
from jax import lax
import numpy as np

D_MODEL = 1024
BATCH = 8
SEQ = 4096
DEPTH = 4

A_HEADS = 8
A_KV_HEADS = 2
A_HEAD_DIM = 64
A_GROUP = A_HEADS // A_KV_HEADS
WINDOW = 128
BLOCK = 128
NUM_BUCKETS = 32
MAX_DISTANCE = 128
R_HEADS = 4
R_QK_DIM = 128
R_V_DIM = 256
CHUNK = 128
ROPE_BASE = 10000.0
D_FF = 2816
CONV_WIDTH = 3
EPS = 1e-6
NEG_INF = -1e30

A_Q = A_HEADS * A_HEAD_DIM
A_KV = A_KV_HEADS * A_HEAD_DIM
R_QK = R_HEADS * R_QK_DIM
R_V = R_HEADS * R_V_DIM
IN_SIZES = (A_Q, A_KV, A_KV, R_QK, R_QK, R_V, R_V, D_MODEL, D_MODEL)
D_IN = sum(IN_SIZES)

kernel_name = "hybrid_swa_sink_retention_convffn_trunk"


def rmsnorm(x, g):
    xf = x.astype(jnp.float32)
    y = xf * lax.rsqrt(jnp.mean(xf * xf, axis=-1, keepdims=True) + EPS)
    return (y * g.astype(jnp.float32)).astype(x.dtype)


def t5_band_buckets():
    i = np.arange(BLOCK)[:, None]
    j = np.arange(2 * BLOCK)[None, :]
    dist = BLOCK + i - j
    n = np.maximum(dist, 0)
    max_exact = NUM_BUCKETS // 2
    large = max_exact + (np.log(np.maximum(n, 1) / max_exact) / math.log(MAX_DISTANCE / max_exact)
                         * (NUM_BUCKETS - max_exact)).astype(np.int32)
    large = np.minimum(large, NUM_BUCKETS - 1)
    bucket = np.where(n < max_exact, n, large).astype(np.int32)
    valid = (dist >= 0) & (dist < WINDOW)
    return bucket, valid


def sliding_window_attention(q, k, v, sinks, rel_table):
    b, s, _ = q.shape
    nb = s // BLOCK
    qb = q.reshape(b, nb, BLOCK, A_KV_HEADS, A_GROUP, A_HEAD_DIM)
    kb = k.reshape(b, nb, BLOCK, A_KV_HEADS, A_HEAD_DIM)
    vb = v.reshape(b, nb, BLOCK, A_KV_HEADS, A_HEAD_DIM)
    pad = ((0, 0), (1, 0), (0, 0), (0, 0), (0, 0))
    k_band = jnp.concatenate([jnp.pad(kb[:, :-1], pad), kb], axis=2)
    v_band = jnp.concatenate([jnp.pad(vb[:, :-1], pad), vb], axis=2)
    scores = jnp.einsum('bnqkgd,bnskd->bkgnqs', qb, k_band).astype(jnp.float32) * (A_HEAD_DIM ** -0.5)
    bucket, valid = t5_band_buckets()
    bias = rel_table.astype(jnp.float32)[jnp.asarray(bucket)]
    bias = jnp.transpose(bias, (2, 0, 1)).reshape(A_KV_HEADS, A_GROUP, BLOCK, 2 * BLOCK)
    block_idx = jnp.arange(nb)[:, None, None]
    key_in_cur = (jnp.arange(2 * BLOCK) >= BLOCK)[None, None, :]
    mask = jnp.asarray(valid)[None] & ((block_idx > 0) | key_in_cur)
    scores = jnp.where(mask[None, None, None], scores + bias[None, :, :, None], NEG_INF)
    sink = sinks.astype(jnp.float32).reshape(A_KV_HEADS, A_GROUP)[None, :, :, None, None, None]
    m = jnp.maximum(jnp.max(scores, axis=-1, keepdims=True), sink)
    p = jnp.exp(scores - m)
    probs = p / (jnp.sum(p, axis=-1, keepdims=True) + jnp.exp(sink - m))
    out = jnp.einsum('bkgnqs,bnskd->bnqkgd', probs.astype(v.dtype), v_band)
    return out.reshape(b, s, A_Q)


def rotate(x, pos):
    half = x.shape[-1] // 2
    freqs = ROPE_BASE ** (-jnp.arange(half, dtype=jnp.float32) / half)
    ang = pos.astype(jnp.float32)[:, None] * freqs[None, :]
    cos = jnp.cos(ang)[None, :, None, :]
    sin = jnp.sin(ang)[None, :, None, :]
    x1, x2 = x[..., :half], x[..., half:]
    return jnp.concatenate([x1 * cos - x2 * sin, x1 * sin + x2 * cos], axis=-1).astype(x.dtype)


def retention(q, k, v, g, norm_gain):
    b, s, _ = q.shape
    nc = s // CHUNK
    pos = jnp.arange(s)
    q = rotate(q.reshape(b, s, R_HEADS, R_QK_DIM), pos)
    k = rotate(k.reshape(b, s, R_HEADS, R_QK_DIM), pos) * (R_QK_DIM ** -0.5)
    vh = v.reshape(b, s, R_HEADS, R_V_DIM)
    to_chunks = lambda t: jnp.transpose(t.reshape(b, nc, CHUNK, R_HEADS, t.shape[-1]), (0, 3, 1, 2, 4))
    qc, kc, vc = to_chunks(q), to_chunks(k), to_chunks(vh)
    log_gamma = jnp.log(1.0 - 2.0 ** (-5.0 - jnp.arange(R_HEADS, dtype=jnp.float32)))
    idx = jnp.arange(CHUNK, dtype=jnp.float32)
    rel = idx[:, None] - idx[None, :]
    decay_intra = jnp.where(rel >= 0, jnp.exp(log_gamma[:, None, None] * jnp.maximum(rel, 0.0)), 0.0)
    qk = jnp.einsum('bhncd,bhnsd->bhncs', qc, kc) * decay_intra[None, :, None]
    y_intra = jnp.einsum('bhncs,bhnse->bhnce', qk, vc)
    zeta = jnp.exp(log_gamma[:, None] * (CHUNK - 1 - idx)[None, :])
    kv_chunks = jnp.einsum('bhncd,bhnce->nbhde', kc * zeta[None, :, None, :, None], vc)
    gamma_c = jnp.exp(log_gamma * CHUNK)[None, :, None, None]

    def step(state, kv):
        return gamma_c * state + kv, state

    init = jnp.zeros(kv_chunks.shape[1:], kv_chunks.dtype)
    _, state_prev = lax.scan(step, init, kv_chunks)
    xi = jnp.exp(log_gamma[:, None] * (idx + 1.0)[None, :])
    y_cross = jnp.einsum('bhncd,nbhde->bhnce', qc, state_prev) * xi[None, :, None, :, None]
    y = jnp.transpose(y_intra + y_cross, (0, 2, 3, 1, 4)).reshape(b, s, R_HEADS, R_V_DIM)
    yf = y.astype(jnp.float32)
    mu = jnp.mean(yf, axis=-1, keepdims=True)
    var = jnp.mean((yf - mu) ** 2, axis=-1, keepdims=True)
    yn = ((yf - mu) * lax.rsqrt(var + EPS)).reshape(b, s, R_V) * norm_gain.astype(jnp.float32)
    return (jax.nn.silu(g.astype(jnp.float32)) * yn).astype(v.dtype)


def conv_ffn(h, w_up, conv_w, conv_b, w_down):
    s = h.shape[1]
    u = h @ w_up
    a, val = u[..., :D_FF], u[..., D_FF:]
    a_pad = jnp.pad(a, ((0, 0), (CONV_WIDTH - 1, 0), (0, 0)))
    a_conv = conv_b + sum(conv_w[t] * a_pad[:, t:t + s] for t in range(CONV_WIDTH))
    return (jax.nn.gelu(a_conv) * val) @ w_down


def setup_inputs(seed: int = 0) -> dict:
    key = jax.random.key(seed)
    ks = jax.random.split(key, 20)
    nrm = lambda k, shape, scale: jax.random.normal(k, shape, jnp.float32) * scale
    gain = lambda k, shape: 1.0 + 0.05 * jax.random.normal(k, shape, jnp.float32)
    return {
        "x": nrm(ks[0], (BATCH, SEQ, D_MODEL), 1.0),
        "norm_pre_mix": gain(ks[1], (DEPTH, D_MODEL)),
        "w_in": nrm(ks[2], (DEPTH, D_MODEL, D_IN), D_MODEL ** -0.5),
        "sinks": nrm(ks[3], (DEPTH, A_HEADS), 0.5),
        "rel_bias": nrm(ks[4], (NUM_BUCKETS, A_HEADS), 0.5),
        "ret_norm": gain(ks[5], (DEPTH, R_V)),
        "w_out_a": nrm(ks[6], (DEPTH, A_Q, D_MODEL), A_Q ** -0.5),
        "w_out_r": nrm(ks[7], (DEPTH, R_V, D_MODEL), R_V ** -0.5),
        "w_out": nrm(ks[8], (DEPTH, D_MODEL, D_MODEL), D_MODEL ** -0.5),
        "norm_post_mix": gain(ks[9], (DEPTH, D_MODEL)),
        "norm_pre_ffn": gain(ks[10], (DEPTH, D_MODEL)),
        "w_up": nrm(ks[11], (DEPTH, D_MODEL, 2 * D_FF), D_MODEL ** -0.5),
        "conv_w": nrm(ks[12], (DEPTH, CONV_WIDTH, D_FF), CONV_WIDTH ** -0.5),
        "conv_b": nrm(ks[13], (DEPTH, D_FF), 0.02),
        "w_down": nrm(ks[14], (DEPTH, D_FF, D_MODEL), D_FF ** -0.5),
        "norm_post_ffn": gain(ks[15], (DEPTH, D_MODEL)),
    }


def reference(x, norm_pre_mix, w_in, sinks, rel_bias, ret_norm, w_out_a, w_out_r, w_out,
              norm_post_mix, norm_pre_ffn, w_up, conv_w, conv_b, w_down, norm_post_ffn):
    split_points = list(np.cumsum(IN_SIZES)[:-1])
    for l in range(DEPTH):
        h = rmsnorm(x, norm_pre_mix[l])
        proj = h @ w_in[l]
        qa, ka, va, qr, kr, vr, gr, gate_a, gate_r = jnp.split(proj, split_points, axis=-1)
        y_a = sliding_window_attention(qa, ka, va, sinks[l], rel_bias) @ w_out_a[l]
        y_r = retention(qr, kr, vr, gr, ret_norm[l]) @ w_out_r[l]
        merged = jax.nn.sigmoid(gate_a) * y_a + jax.nn.sigmoid(gate_r) * y_r
        x = x + rmsnorm(merged @ w_out[l], norm_post_mix[l])
        h = rmsnorm(x, norm_pre_ffn[l])
        x = x + rmsnorm(conv_ffn(h, w_up[l], conv_w[l], conv_b[l], w_down[l]), norm_post_ffn[l])
    return x
```

```python
import math
from contextlib import ExitStack
import numpy as np
import concourse.bass as bass
import concourse.mybir as mybir
from concourse.bass_utils import run_bass_kernel_spmd

F32, BF16 = mybir.dt.float32, mybir.dt.bfloat16
ALU, AF = mybir.AluOpType, mybir.ActivationFunctionType

D = 1024
SEQ = 4096
DEPTH = 4
T = 512
NB = 4
A_HEADS, A_KV, A_HD = 8, 2, 64
R_HEADS, R_DK, R_DV = 4, 128, 256
D_FF = 2816
NF = 22
NUM_BUCKETS, MAX_DISTANCE = 32, 128
EPS = 1e-6
NEG = -30000.0
SEM_LIMIT = 30000
NWS = 4
DEBUG_TAGS = None
STOP_AFTER = 99


class _Stop(Exception):
    pass

WSLOT = 4096

W1_GROUPS = [
    [("qa", 0), ("qa", 1), ("qa", 2), ("qa", 3)],
    [("ka", 0), ("va", 0), ("vr", 0), ("vr", 1)],
    [("vr", 2), ("vr", 3), ("vr", 4), ("vr", 5)],
    [("vr", 6), ("vr", 7), ("gr", 0), ("gr", 1)],
    [("qr", 0), ("qrs", 0), ("kr", 0), ("krs", 0)],
    [("qr", 1), ("qrs", 1), ("kr", 1), ("krs", 1)],
    [("qr", 2), ("qrs", 2), ("kr", 2), ("krs", 2)],
    [("qr", 3), ("qrs", 3), ("kr", 3), ("krs", 3)],
    [("gr", 2), ("gr", 3), ("gr", 4), ("gr", 5)],
    [("gr", 6), ("gr", 7)],
]


def _layer_groups():
    gs = []
    for roles in W1_GROUPS:
        gs.append(("w1", roles, 8 * 128 * len(roles)))
    for m in range(8):
        gs.append(("m", m, 8 * 128 * 3 + 4 * 128))
    for mb in range(2):
        gs.append(("wo", mb, 4096))
    for j in range(11):
        gs.append(("up", j, 4096))
    for m in range(8):
        gs.append(("dn", m, NF * 128))
    return gs


LAYER_GROUPS = _layer_groups()
WTOT = sum(g[2] for g in LAYER_GROUPS)


def _w1_cols(role, idx):
    A_Q, A_K = 512, 128
    o_qa, o_ka, o_va = 0, 512, 640
    o_qr, o_kr, o_vr, o_gr = 768, 1280, 1792, 2816
    ar = np.arange
    if role == "qa":
        return np.concatenate([o_qa + idx * 64 + ar(64), o_qa + (4 + idx) * 64 + ar(64)])
    if role == "ka":
        return o_ka + ar(128)
    if role == "va":
        return o_va + ar(128)
    if role == "qr":
        return o_qr + idx * 128 + ar(128)
    if role == "qrs":
        return o_qr + idx * 128 + np.concatenate([64 + ar(64), ar(64)])
    if role == "kr":
        return o_kr + idx * 128 + ar(128)
    if role == "krs":
        return o_kr + idx * 128 + np.concatenate([64 + ar(64), ar(64)])
    if role == "vr":
        return o_vr + idx * 128 + ar(128)
    if role == "gr":
        return o_gr + idx * 128 + ar(128)
    raise ValueError(role)


def _kchunks(w):
    K, n = w.shape
    return np.ascontiguousarray(w.reshape(K // 128, 128, n).transpose(1, 0, 2)).reshape(128, -1)


def _pack_layer(w_in, w_out_a, w_out_r, w_out, w_up, w_down):
    o_ga, o_gg = 3840, 4864
    parts = []
    rows_a = np.concatenate([np.concatenate([c * 64 + np.arange(64), (4 + c) * 64 + np.arange(64)]) for c in range(4)])
    w_oa = w_out_a[rows_a]
    for kind, arg, size in LAYER_GROUPS:
        if kind == "w1":
            cols = np.concatenate([_w1_cols(r, i) for r, i in arg])
            parts.append(_kchunks(w_in[:, cols]))
        elif kind == "m":
            m = arg
            cs = slice(m * 128, (m + 1) * 128)
            parts.append(np.concatenate([
                _kchunks(w_out_r[:, cs]),
                _kchunks(w_in[:, o_ga + m * 128:o_ga + (m + 1) * 128]),
                _kchunks(w_in[:, o_gg + m * 128:o_gg + (m + 1) * 128]),
                _kchunks(w_oa[:, cs]),
            ], axis=1))
        elif kind == "wo":
            parts.append(_kchunks(w_out[:, arg * 512:(arg + 1) * 512]))
        elif kind == "up":
            j = arg
            cols = np.concatenate([np.arange(2 * j * 128, (2 * j + 2) * 128), D_FF + np.arange(2 * j * 128, (2 * j + 2) * 128)])
            parts.append(_kchunks(w_up[:, cols]))
        elif kind == "dn":
            parts.append(_kchunks(w_down[:, arg * 128:(arg + 1) * 128]))
    out = np.concatenate(parts, axis=1)
    assert out.shape == (128, WTOT), out.shape
    return out


def _t5_bucket(n):
    max_exact = NUM_BUCKETS // 2
    n = np.maximum(n, 0)
    large = max_exact + (np.log(np.maximum(n, 1) / max_exact) / math.log(MAX_DISTANCE / max_exact)
                         * (NUM_BUCKETS - max_exact)).astype(np.int32)
    large = np.minimum(large, NUM_BUCKETS - 1)
    return np.where(n < max_exact, n, large).astype(np.int32)


def _constants(n_tok):
    c = {}
    oh = np.zeros((33, 2 * 255), np.float32)
    for pc in range(2):
        for i in range(255):
            dist = (127 - i) if pc == 1 else (255 - i)
            valid = (dist >= 0) and (dist < 128)
            b = int(_t5_bucket(np.array([dist]))[0]) if valid else 32
            oh[b, pc * 255 + i] = 1.0
    c["oh"] = oh
    lg = np.log(1.0 - 2.0 ** (-5.0 - np.arange(R_HEADS, dtype=np.float64)))
    idx = np.arange(128, dtype=np.float64)
    sc = R_DK ** -0.5
    rel = idx[None, :] - idx[:, None]
    dec = np.where(rel >= 0, np.exp(lg[:, None, None] * np.maximum(rel, 0.0)), 0.0) * sc
    c["decay"] = np.ascontiguousarray(dec.transpose(1, 0, 2)).reshape(128, 512).astype(np.float32)
    xi = np.exp(lg[:, None] * (idx + 1.0)[None, :])
    c["xib"] = np.ascontiguousarray(np.broadcast_to(xi.reshape(1, 512), (128, 512))).astype(np.float32)
    zeta = np.exp(lg[:, None] * (127 - idx)[None, :]) * sc
    c["zeta"] = np.ascontiguousarray(zeta.T).astype(np.float32)
    c["ident"] = np.eye(128, dtype=np.float32)
    half = 64
    freqs = (10000.0 ** (-np.arange(half, dtype=np.float32) / half)).astype(np.float32)
    ang = (np.arange(n_tok, dtype=np.float32)[None, :] * freqs[:, None]).astype(np.float32)
    cs, sn = np.cos(ang.astype(np.float64)), np.sin(ang.astype(np.float64))
    c["cosT"] = np.concatenate([cs, cs], axis=0).astype(np.float32)
    c["sinT"] = np.concatenate([-sn, sn], axis=0).astype(np.float32)
    return c


GAMMA_C = [float((1.0 - 2.0 ** (-5.0 - h)) ** 128) for h in range(R_HEADS)]


class Buf:
    __slots__ = ("name", "w", "r")

    def __init__(self, name):
        self.name = name
        self.w = None
        self.r = {}


class Prog:
    ENG = ("pe", "act", "dve", "pool", "sp")
    BLK = {"pe": "tensor", "act": "scalar", "dve": "vector", "pool": "gpsimd", "sp": "sync"}

    def __init__(self):
        self.ops = {e: [] for e in self.ENG}
        self.cnt = {}
        self.seen = {e: {} for e in self.ENG}
        self.epoch = {e: 0 for e in self.ENG}
        self.debug_tags = None

    def _need(self, e, ts, waits):
        if ts is None:
            return
        k, v = ts
        if self.seen[e].get(k, 0) >= v:
            return
        assert self.cnt.get(k, 0) >= v, ("unresolved timestamp", k, v)
        if e == "pe" and isinstance(k, tuple) and k[0] == "pe":
            return
        waits[k] = max(waits.get(k, 0), v)

    @staticmethod
    def _flat(x):
        out = []
        for b in x:
            if isinstance(b, Buf):
                out.append(b)
            else:
                out.extend(Prog._flat(b))
        return out

    def _waits(self, e, reads, writes):
        waits = {}
        for b in reads:
            self._need(e, b.w, waits)
        for b in writes:
            self._need(e, b.w, waits)
            for k, v in b.r.items():
                self._need(e, (k, v), waits)
        for k, v in waits.items():
            self.seen[e][k] = v
        return list(waits.items())

    def _mark(self, ts, reads, writes):
        k, v = ts
        for b in reads:
            if b.r.get(k, 0) < v:
                b.r[k] = v
        for b in writes:
            b.w = ts
            b.r = {}

    def op(self, e, fn, reads=(), writes=()):
        reads, writes = self._flat(reads), self._flat(writes)
        waits = self._waits(e, reads, writes)
        k = (e, self.epoch[e])
        self.cnt[k] = self.cnt.get(k, 0) + 1
        ts = (k, self.cnt[k])
        if self.cnt[k] >= SEM_LIMIT:
            self.epoch[e] += 1
        import sys as _s
        fr = _s._getframe(1)
        tags = []
        while fr is not None and len(tags) < 3:
            tags.append(fr.f_lineno)
            fr = fr.f_back
        self.ops[e].append((waits, fn, (k, 1), tags))
        self._mark(ts, reads, writes)

    def dma(self, q, fn, semkey, reads=(), writes=()):
        reads, writes = self._flat(reads), self._flat(writes)
        waits = self._waits(q, reads, writes)
        self.cnt[semkey] = self.cnt.get(semkey, 0) + 16
        ts = (semkey, self.cnt[semkey])
        self.ops[q].append((waits, fn, (semkey, 16), None))
        self._mark(ts, reads, writes)

    def wait_all(self, e, bufs):
        waits = self._waits(e, bufs, ())
        self.ops[e].append((waits, None, None, None))

    def emit(self, nc, es):
        keys = list(self.cnt.keys())
        sems = {}
        for i, k in enumerate(keys):
            sems[k] = es.enter_context(nc.semaphore("s%d" % i))
        block = es.enter_context(nc.Block())
        for e in self.ENG:
            oplist = self.ops[e]

            def body(eng, oplist=oplist):
                for waits, fn, inc, tags in oplist:
                    for k, v in waits:
                        eng.wait_ge(sems[k], v)
                    if fn is None:
                        continue
                    ins = fn(eng)
                    ins.then_inc(sems[inc[0]], inc[1])
                    if self.debug_tags is not None:
                        try:
                            self.debug_tags[str(ins.ins.name)] = tags
                        except Exception:
                            pass

            getattr(block, self.BLK[e])(body)


def build(n_tiles=8, depth=DEPTH):
    n_tok = n_tiles * T
    nc = bass.Bass("TRN2", target_bir_lowering=False)
    dr = {}

    def din(name, shape):
        dr[name] = nc.dram_tensor(name, list(shape), F32, kind="ExternalInput").ap()
        return dr[name]

    xT_d = din("xT", [D, n_tok])
    wall_d = din("wall", [depth, 128, WTOT])
    pvec_d = din("pvec", [128, depth * 128])
    sinkb_d = din("sinkb", [128, depth * 8])
    relb_d = din("relb", [32, 8])
    oh_d = din("oh", [33, 510])
    decay_d = din("decay", [128, 512])
    xib_d = din("xib", [128, 512])
    zeta_d = din("zeta", [128, 4])
    ident_d = din("ident", [128, 128])
    cos_d = din("cosT", [128, n_tok])
    sin_d = din("sinT", [128, n_tok])
    yT_d = nc.dram_tensor("yT", [D, n_tok], F32, kind="ExternalOutput").ap()

    P = Prog()
    if DEBUG_TAGS is not None:
        P.debug_tags = DEBUG_TAGS
    es = ExitStack()
    with es:
        def sb(name, shape, dt):
            return es.enter_context(nc.sbuf_tensor("s_" + name, list(shape), dt))

        def ps(name, shape, dt):
            return es.enter_context(nc.psum_tensor("p_" + name, list(shape), dt))

        xT = sb("xT", [128, 8, T], F32); B_x = [Buf("xT%d" % c) for c in range(8)]
        S32 = sb("S32", [128, depth * 4, 256], F32); B_S32 = [[Buf("S32") for _ in range(4)] for _ in range(depth)]
        Sbf = sb("Sbf", [128, 4, 256], BF16); B_Sbf = [Buf("Sbf") for _ in range(4)]
        kaT = sb("kaT", [128, depth, 640], BF16); B_ka = [Buf("ka") for _ in range(depth)]
        vaT = sb("vaT", [128, depth * 5, 128], BF16); B_va = [Buf("va") for _ in range(depth)]
        ccar = sb("ccar", [128, depth * NF, 2], F32); B_cc = [Buf("cc") for _ in range(depth)]
        bias = sb("bias", [128, 2 * 8, 128], F32); B_bias = Buf("bias")
        decay = sb("decay", [128, 512], F32); B_decay = Buf("decay")
        xib = sb("xib", [128, 512], F32); B_xib = Buf("xib")
        zeta = sb("zeta", [128, 4], F32); B_zeta = Buf("zeta")
        pvec = sb("pvec", [128, depth * 128], F32); B_pvec = Buf("pvec")
        esink = sb("esink", [128, depth * 8], F32); B_es = Buf("esink")
        identb = sb("identb", [128, 128], BF16); B_ident = Buf("ident")
        onesb = sb("onesb", [128, 128], BF16); B_ones = Buf("ones")
        inv256 = sb("inv256", [128, 128], BF16)
        cosS = sb("cosS", [128, T], F32); B_cos = Buf("cos")
        sinS = sb("sinS", [128, T], F32); B_sin = Buf("sin")
        wsl = [sb("ws%d" % i, [128, WSLOT], BF16) for i in range(NWS)]
        B_ws = [Buf("ws%d" % i) for i in range(NWS)]
        hT = sb("hT", [128, 8, T], BF16); B_h = Buf("hT")
        ar3 = sb("ar3", [128, 8 * T], BF16)
        sq = ar3[:, :].rearrange("p (c t) -> p c t", c=8)
        qaT = ar3[:, 0:4 * T].rearrange("p (c t) -> p c t", c=4)
        qrT = ar3[:, 4 * T:8 * T].rearrange("p (c t) -> p c t", c=4)
        B_sq = [Buf("sq%d" % c) for c in range(8)]
        B_qa = B_sq[0:4]
        B_qr = B_sq[4:8]
        ar1 = sb("ar1", [128, NF * T], BF16)
        gated = ar1[:, :].rearrange("p (f t) -> p f t", f=NF)
        q2T = ar1[:, 0:2048].rearrange("p (c t) -> p c t", c=4)
        krT = ar1[:, 2048:4096].rearrange("p (c t) -> p c t", c=4)
        PTa = [ar1[:, 4096 + i * 1024: 4096 + (i + 1) * 1024].rearrange("p (c t) -> p c t", c=2) for i in range(2)]
        PTr = ar1[:, 6144:6656]
        Kz = ar1[:, 6656:7168].rearrange("p (n d) -> p n d", n=4)
        yaT = ar1[:, 7168:9216].rearrange("p (c t) -> p c t", c=4)
        ysb = ar1[:, 9216:10240].rearrange("p (c t) -> p c t", c=2)
        ysq = ar1[:, 10240:11264].rearrange("p (c t) -> p c t", c=2)
        B_gated = Buf("gated"); B_q2 = Buf("q2"); B_kr = Buf("kr"); B_PTa = [Buf("PTa0"), Buf("PTa1")]
        B_PTr = Buf("PTr"); B_Kz = Buf("Kz"); B_ya = Buf("ya"); B_ysb = Buf("ysb"); B_ysq = Buf("ysq")
        G_ar1 = (B_gated, B_q2, B_kr, B_PTa[0], B_PTa[1], B_PTr, B_Kz, B_ya, B_ysb, B_ysq)
        tmpA = sb("tmpA", [128, T], F32); B_tA = Buf("tmpA")
        tmpB = sb("tmpB", [128, T], F32); B_tB = Buf("tmpB")
        arV = sb("arV", [128, 4096], BF16)
        vr = arV[:, :].rearrange("p (n e) -> p n e", n=4)
        merged = arV[:, :].rearrange("p (c t) -> p c t", c=8)
        B_vr = Buf("vr"); B_mg = Buf("merged")
        silu = sb("silu", [128, 8, T], BF16); B_silu = Buf("silu")
        yrT = sb("yrT", [128, 8, T], BF16); B_yr = Buf("yr")
        gtmp = [sb("gtmp%d" % i, [128, T], BF16) for i in range(2)]; B_gt = [Buf("gt%d" % i) for i in range(2)]
        arF = sb("arF", [128, 8 * T], F32)
        o_sb = arF[:, :].rearrange("p (c t) -> p c t", c=8)
        a_sb = [arF[:, i * 516: i * 516 + T + 2] for i in range(2)]
        acc = [arF[:, 1032 + i * T: 1032 + (i + 1) * T] for i in range(2)]
        gl = [arF[:, 2056 + i * T: 2056 + (i + 1) * T] for i in range(2)]
        B_o = [Buf("o%d" % c) for c in range(8)]
        B_asb = [Buf("asb%d" % i) for i in range(2)]; B_acc = [Buf("acc%d" % i) for i in range(2)]; B_gl = [Buf("gl%d" % i) for i in range(2)]
        G_ffn = tuple(B_asb + B_acc + B_gl)
        G_o = tuple(B_o)
        rstd = sb("rstd", [128, T], F32); B_rstd = Buf("rstd")
        mean_sb = sb("mean_sb", [128, T], F32); B_mean = Buf("mean")
        tab = sb("tab", [33, 8], F32)
        tabh = sb("tabh", [33, 8], BF16)
        tabh32 = sb("tabh32", [33, 8], F32)
        tabl = sb("tabl", [33, 8], BF16)
        ohb = sb("ohb", [33, 510], BF16); B_oh = Buf("oh")
        B_tab = Buf("tab")
        NPB = 7
        pbank = [ps("pb%d" % i, [128, 512], F32) for i in range(NPB)]
        B_pb = [Buf("pb%d" % i) for i in range(NPB)]
        ptr = ps("ptr", [128, 1024], BF16); B_ptr = Buf("ptr")
        pstate = {"i": 0, "live": set()}

        def nbank():
            for d_ in range(NPB):
                i = (pstate["i"] + d_) % NPB
                if i not in pstate["live"]:
                    pstate["i"] = (i + 1) % NPB
                    pstate["live"].add(i)
                    return pbank[i], B_pb[i]
            raise RuntimeError("no free PSUM bank")

        def rel(*bs):
            for b_ in bs:
                pstate["live"].discard(B_pb.index(b_))

        def mmgroup(specs, reads, writes):
            def fn(eng, specs=specs):
                ins = None
                for (o, l_, r_, st, sp_) in specs:
                    ins = eng.matmul(o, lhsT=l_, rhs=r_, start=st, stop=sp_)
                return ins
            P.op("pe", fn, reads, writes)

        def transposes(specs, reads, writes):
            def fn(eng, specs=specs):
                ins = None
                for (o, i_) in specs:
                    ins = eng.transpose(out=o, in_=i_, identity=identb[:])
                return ins
            P.op("pe", fn, reads, writes)

        def act(out, in_, func, reads, writes, scale=None):
            def fn(eng):
                if scale is None:
                    return eng.activation(out=out, in_=in_, func=func)
                return eng.activation(out=out, in_=in_, func=func, scale=scale)
            P.op("act", fn, reads, writes)

        def act_mul(out, in_, mul, reads, writes):
            P.op("act", lambda eng: eng.mul(out=out, in_=in_, mul=mul), reads, writes)

        def act_copy(out, in_, reads, writes):
            P.op("act", lambda eng: eng.copy(out=out, in_=in_), reads, writes)

        def tt(e, out, in0, in1, op, reads, writes):
            P.op(e, lambda eng: eng.tensor_tensor(out=out, in0=in0, in1=in1, op=op), reads, writes)

        def ts2(e, out, in0, s1, s2, op0, op1, reads, writes):
            P.op(e, lambda eng: eng.tensor_scalar(out=out, in0=in0, scalar1=s1, scalar2=s2, op0=op0, op1=op1), reads, writes)

        def stt(e, out, in0, scalar, in1, op0, op1, reads, writes):
            P.op(e, lambda eng: eng.scalar_tensor_tensor(out=out, in0=in0, scalar=scalar, in1=in1, op0=op0, op1=op1), reads, writes)

        def cpy(e, out, in_, reads, writes):
            P.op(e, lambda eng: eng.tensor_copy(out=out, in_=in_), reads, writes)

        def recip(out, in_, reads, writes):
            P.op("dve", lambda eng: eng.reciprocal(out=out, in_=in_), reads, writes)

        def memset(e, ap, val, writes):
            P.op(e, lambda eng: eng.memset(ap, val), (), writes)

        def dma(q, out, in_, semkey, reads, writes):
            P.dma(q, lambda eng: eng.dma_start(out=out, in_=in_), semkey, reads, writes)

        gseq = []
        for t in range(n_tiles):
            for l in range(depth):
                off = 0
                for (kind, arg, size) in LAYER_GROUPS:
                    gseq.append((l, off, size))
                    off += size
        wstate = {"issued": 0, "used": 0}

        def issue_w():
            g = wstate["issued"]
            if g >= len(gseq):
                return
            l_, off, size = gseq[g]
            s_ = g % NWS
            dma("pool", wsl[s_][:, 0:size], wall_d[l_, :, off:off + size], "w%d" % s_, (), (B_ws[s_],))
            wstate["issued"] = g + 1

        def next_w():
            g = wstate["used"]
            wstate["used"] = g + 1
            while wstate["issued"] < min(g + NWS, len(gseq)):
                issue_w()
            s_ = g % NWS
            return wsl[s_], B_ws[s_]

        dma("sp", decay[:], decay_d, "su0", (), (B_decay,))
        dma("sp", xib[:], xib_d, "su1", (), (B_xib,))
        dma("sp", zeta[:], zeta_d, "su2", (), (B_zeta,))
        dma("sp", pvec[:], pvec_d, "su3", (), (B_pvec,))
        dma("sp", esink[:], sinkb_d, "su4", (), (B_es,))
        dma("sp", tab[0:32, :], relb_d, "su5", (), (B_tab,))
        dma("pool", identb[:], ident_d, "su6", (), (B_ident,))
        dma("pool", ohb[:], oh_d, "su7", (), (B_oh,))
        for _ in range(NWS - 1):
            issue_w()
        memset("dve", onesb[:], 1.0, (B_ones,))
        memset("dve", inv256[:], 1.0 / 256.0, (B_ones,))
        memset("dve", S32[:], 0.0, B_S32)
        memset("dve", kaT[:], 0.0, B_ka)
        memset("dve", vaT[:], 0.0, B_va)
        memset("dve", ccar[:], 0.0, B_cc)
        memset("dve", tab[32:33, :], NEG, (B_tab,))
        act(esink[:], esink[:], AF.Exp, (B_es,), (B_es,))
        cpy("dve", tabh[:], tab[:], (B_tab,), (B_tab,))
        cpy("dve", tabh32[:], tabh[:], (B_tab,), (B_tab,))
        tt("dve", tabh32[:], tab[:], tabh32[:], ALU.subtract, (B_tab,), (B_tab,))
        cpy("dve", tabl[:], tabh32[:], (B_tab,), (B_tab,))
        for b4 in range(4):
            pc, q0 = b4 // 2, (b4 % 2) * 64
            pb_, Bp = nbank()
            specs = []
            for qq in range(64):
                q = q0 + qq
                lh = ohb[:, pc * 255 + 127 - q: pc * 255 + 255 - q]
                specs.append((pb_[:, qq * 8:(qq + 1) * 8], lh, tabh[:], True, False))
                specs.append((pb_[:, qq * 8:(qq + 1) * 8], lh, tabl[:], False, True))
            mmgroup(specs, (B_tab, B_oh), (Bp,))
            cpy("dve", bias[:, pc * 8:(pc + 1) * 8, q0:q0 + 64], pb_[:, :].rearrange("p (q h) -> p h q", h=8), (Bp,), (B_bias,))
            rel(Bp)

        def gvec(l, j):
            return pvec[:, l * 128 + j * 8: l * 128 + (j + 1) * 8]

        def rms_rstd():
            pb_, Bp = nbank()
            specs = [(pb_[:, :], onesb[:], sq[:, c, :], c == 0, c == 7) for c in range(8)]
            mmgroup(specs, (B_sq, B_ones), (Bp,))
            ts2("dve", rstd[:], pb_[:, :], 1.0 / D, EPS, ALU.mult, ALU.add, (Bp,), (B_rstd,))
            rel(Bp)
            act(rstd[:], rstd[:], AF.Sqrt, (B_rstd,), (B_rstd,))
            recip(rstd[:], rstd[:], (B_rstd,), (B_rstd,))

        def prenorm(l, j):
            for c in range(8):
                act(sq[:, c, :], xT[:, c, :], AF.Square, (B_x[c],), (B_sq[c],))
            rms_rstd()
            g = gvec(l, j)
            for c in range(8):
                stt("dve", hT[:, c, :], xT[:, c, :], g[:, c:c + 1], rstd[:], ALU.mult, ALU.mult, (B_x[c], B_rstd, B_pvec), (B_h,))

        def postnorm_residual(l, j):
            rms_rstd()
            g = gvec(l, j)
            for c in range(8):
                stt("dve", o_sb[:, c, :], o_sb[:, c, :], g[:, c:c + 1], rstd[:], ALU.mult, ALU.mult, (B_o[c], B_rstd, B_pvec), (B_o[c],))
                tt("pool", xT[:, c, :], xT[:, c, :], o_sb[:, c, :], ALU.add, (B_x[c], B_o[c]), (B_x[c],))

        def proj_fm(wv, Bw, j):
            pb_, Bp = nbank()
            specs = [(pb_[:, :], wv[:, k, j * 128:(j + 1) * 128], hT[:, k, :], k == 0, k == 7) for k in range(8)]
            mmgroup(specs, (Bw, B_h), (Bp,))
            return pb_, Bp

        def proj_tm(wv, Bw, j):
            pb_, Bp = nbank()
            specs = []
            for n in range(NB):
                for k in range(8):
                    specs.append((pb_[:, n * 128:(n + 1) * 128], hT[:, k, n * 128:(n + 1) * 128], wv[:, k, j * 128:(j + 1) * 128], k == 0, k == 7))
            mmgroup(specs, (Bw, B_h), (Bp,))
            return pb_, Bp

        def do_w1_group(l, roles):
            w, Bw = next_w()
            nch = len(roles)
            wv = w[:, 0:8 * 128 * nch].rearrange("p (k n) -> p k n", k=8)
            j = 0
            while j < nch:
                role, idx = roles[j]
                if role == "qa":
                    pb_, Bp = proj_fm(wv, Bw, j)
                    act_mul(qaT[:, idx, :], pb_[:, :], A_HD ** -0.5, (Bp,), (B_qa[idx],))
                    rel(Bp)
                elif role == "ka":
                    pb_, Bp = proj_fm(wv, Bw, j)
                    act_copy(kaT[:, l, 128:640], pb_[:, :], (Bp,), (B_ka[l],))
                    rel(Bp)
                elif role == "va":
                    pb_, Bp = proj_tm(wv, Bw, j)
                    act_copy(vaT[:, l * 5 + 1:l * 5 + 5, :], pb_[:, :].rearrange("p (n c) -> p n c", n=4), (Bp,), (B_va[l],))
                    rel(Bp)
                elif role == "vr":
                    pb_, Bp = proj_tm(wv, Bw, j)
                    act_copy(vr[:, :, idx * 128:(idx + 1) * 128], pb_[:, :].rearrange("p (n c) -> p n c", n=4), (Bp,), (B_vr, B_mg))
                    rel(Bp)
                elif role == "gr":
                    pb_, Bp = proj_fm(wv, Bw, j)
                    act(silu[:, idx, :], pb_[:, :], AF.Silu, (Bp,), (B_silu,))
                    rel(Bp)
                elif role in ("qr", "kr"):
                    pq, Bq = proj_fm(wv, Bw, j)
                    pq2, Bq2 = proj_fm(wv, Bw, j + 1)
                    tt("dve", tmpA[:], pq[:, :], cosS[:], ALU.mult, (Bq, B_cos), (B_tA,))
                    tt("dve", tmpB[:], pq2[:, :], sinS[:], ALU.mult, (Bq2, B_sin), (B_tB,))
                    rel(Bq, Bq2)
                    if role == "qr":
                        tt("pool", qrT[:, idx, :], tmpA[:], tmpB[:], ALU.add, (B_tA, B_tB), (B_qr[idx],))
                        tt("pool", q2T[:, idx, :].rearrange("p (n c) -> p n c", n=4), qrT[:, idx, :].rearrange("p (n c) -> p n c", n=4),
                           bass.AP(xib, idx * 128, [[512, 128], [0, 4], [1, 128]]), ALU.mult, (B_qr[idx], B_xib), (B_q2, B_gated))
                    else:
                        tt("pool", krT[:, idx, :], tmpA[:], tmpB[:], ALU.add, (B_tA, B_tB), (B_kr, B_gated))
                    j += 1
                else:
                    raise ValueError(role)
                j += 1

        for t in range(n_tiles):
            tsl = slice(t * T, (t + 1) * T)
            dma("sp", xT[:], xT_d[:, tsl].rearrange("(c p) t -> p c t", p=128), "xl", (), B_x)
            dma("sp", cosS[:], cos_d[:, tsl], "cl", (), (B_cos,))
            dma("sp", sinS[:], sin_d[:, tsl], "sl", (), (B_sin,))
            for l in range(depth):
              try:
                first_blk = (t == 0)
                if STOP_AFTER <= 0:
                    raise _Stop()
                prenorm(l, 0)
                if STOP_AFTER <= 1:
                    raise _Stop()
                for gi in range(4):
                    do_w1_group(l, W1_GROUPS[gi])
                fillers = [(lambda gi=gi, l=l: do_w1_group(l, W1_GROUPS[gi])) for gi in range(4, len(W1_GROUPS))]
                if STOP_AFTER <= 2:
                    raise _Stop()

                pairs = [(n, j) for n in range(NB) for j in range(A_KV)]

                def att_scores(i):
                    n, j = pairs[i]
                    prs = slice(j * 64, (j + 1) * 64)
                    out = []
                    for pc in range(2):
                        if pc == 0 and first_blk and n == 0:
                            out.append(None)
                            continue
                        pb_, Bp = nbank()
                        kc = slice((n + pc) * 128, (n + pc + 1) * 128)
                        specs = [(pb_[:, :], kaT[prs, l, kc], qaT[prs, :, n * 128:(n + 1) * 128], True, True)]
                        mmgroup(specs, (B_ka[l], B_qa), (Bp,))
                        out.append((pb_, Bp))
                    return out

                def att_rest(i, sc):
                    n, j = pairs[i]
                    prs = slice(j * 64, (j + 1) * 64)
                    pt, Bpt = PTa[i % 2], B_PTa[i % 2]
                    used = []
                    for pc in range(2):
                        if sc[pc] is None:
                            continue
                        pb_, Bp = sc[pc]
                        bview = bias[:, pc * 8 + j * 4: pc * 8 + j * 4 + 4, :]
                        tt("dve", tmpA[:].rearrange("p (c q) -> p c q", c=4), pb_[:, :].rearrange("p (c q) -> p c q", c=4), bview, ALU.add, (Bp, B_bias), (B_tA,))
                        rel(Bp)
                        act(pt[:, pc, :], tmpA[:], AF.Exp, (B_tA,), (Bpt, B_gated))
                        used.append(pc)
                    pn, Bn = nbank()
                    pd, Bd = nbank()
                    specs = []
                    for ii, pc in enumerate(used):
                        specs.append((pn[:, :], vaT[:, l * 5 + n + pc, :], pt[:, pc, :], ii == 0, ii == len(used) - 1))
                    for ii, pc in enumerate(used):
                        specs.append((pd[:, :], onesb[:], pt[:, pc, :], ii == 0, ii == len(used) - 1))
                    mmgroup(specs, (B_va[l], Bpt, B_ones), (Bn, Bd))
                    esv = bass.AP(esink, l * 8 + j * 4, [[depth * 8, 128], [1, 4], [0, 128]])
                    tt("dve", tmpB[:].rearrange("p (c q) -> p c q", c=4), pd[:, :].rearrange("p (c q) -> p c q", c=4), esv, ALU.add, (Bd, B_es), (B_tB,))
                    rel(Bd)
                    recip(tmpB[:], tmpB[:], (B_tB,), (B_tB,))
                    tt("dve", yaT[prs, :, n * 128:(n + 1) * 128], pn[prs, :].rearrange("p (c q) -> p c q", c=4),
                       tmpB[prs, :].rearrange("p (c q) -> p c q", c=4), ALU.mult, (Bn, B_tB), (B_ya, B_gated))
                    rel(Bn)

                sc_next = att_scores(0)
                for i in range(len(pairs)):
                    sc_cur = sc_next
                    if i + 1 < len(pairs):
                        sc_next = att_scores(i + 1)
                    if fillers and i >= 1:
                        fillers.pop(0)()
                    att_rest(i, sc_cur)
                while fillers:
                    fillers.pop(0)()
                if t + 1 < n_tiles:
                    cpy("pool", kaT[:, l, 0:128], kaT[:, l, 512:640], (B_ka[l],), (B_ka[l],))
                    cpy("pool", vaT[:, l * 5, :], vaT[:, l * 5 + 4, :], (B_va[l],), (B_va[l],))

                if STOP_AFTER <= 3:
                    raise _Stop()
                for h in range(R_HEADS):
                    pA, BA = nbank()
                    specs = [(pA[:, n * 128:(n + 1) * 128], krT[:, h, n * 128:(n + 1) * 128], qrT[:, h, n * 128:(n + 1) * 128], True, True) for n in range(NB)]
                    mmgroup(specs, (B_kr, B_qr[h]), (BA,))
                    tt("dve", PTr.rearrange("p (n c) -> p n c", n=4), pA[:, :].rearrange("p (n c) -> p n c", n=4),
                       bass.AP(decay, h * 128, [[512, 128], [0, 4], [1, 128]]), ALU.mult, (BA, B_decay), (B_PTr, B_gated))
                    rel(BA)
                    transposes([(ptr[:, n * 128:(n + 1) * 128], krT[:, h, n * 128:(n + 1) * 128]) for n in range(NB)], (B_kr, B_ident), (B_ptr,))
                    act_mul(Kz.rearrange("p n d -> p (n d)"), ptr[:, 0:512], zeta[:, h:h + 1], (B_ptr, B_zeta), (B_Kz, B_gated))
                    pY0, BY0 = nbank()
                    pY1, BY1 = nbank()
                    pYs = (pY0, pY1)
                    BYs = (BY0, BY1)
                    for n in range(NB):
                        act_copy(Sbf[:, h, :], S32[:, l * 4 + h, :], (B_S32[l][h],), (B_Sbf[h],))
                        specs = []
                        for ec in range(2):
                            o = pYs[ec][:, n * 128:(n + 1) * 128]
                            specs.append((o, vr[:, n, h * 256 + ec * 128: h * 256 + (ec + 1) * 128], PTr[:, n * 128:(n + 1) * 128], True, False))
                            specs.append((o, Sbf[:, h, ec * 128:(ec + 1) * 128], q2T[:, h, n * 128:(n + 1) * 128], False, True))
                        mmgroup(specs, (B_vr, B_PTr, B_Sbf[h], B_q2), (BY0, BY1))
                        pK, BK = nbank()
                        mmgroup([(pK[:, 0:256], Kz[:, n, :], vr[:, n, h * 256:(h + 1) * 256], True, True)], (B_Kz, B_vr), (BK,))
                        stt("dve", S32[:, l * 4 + h, :], S32[:, l * 4 + h, :], GAMMA_C[h], pK[:, 0:256], ALU.mult, ALU.add, (B_S32[l][h], BK), (B_S32[l][h],))
                        rel(BK)
                    for ec in range(2):
                        act_copy(ysb[:, ec, :], pYs[ec][:, :], (BYs[ec],), (B_ysb, B_gated))
                        act(ysq[:, ec, :], pYs[ec][:, :], AF.Square, (BYs[ec],), (B_ysq, B_gated))
                    rel(BY0, BY1)
                    pM, BM = nbank()
                    pQ, BQ = nbank()
                    mmgroup([(pM[:, :], inv256[:], ysb[:, ec, :], ec == 0, ec == 1) for ec in range(2)]
                            + [(pQ[:, :], inv256[:], ysq[:, ec, :], ec == 0, ec == 1) for ec in range(2)], (B_ysb, B_ysq, B_ones), (BM, BQ))
                    act_copy(mean_sb[:], pM[:, :], (BM,), (B_mean,))
                    rel(BM)
                    tt("dve", tmpA[:], mean_sb[:], mean_sb[:], ALU.mult, (B_mean,), (B_tA,))
                    tt("dve", tmpA[:], pQ[:, :], tmpA[:], ALU.subtract, (BQ, B_tA), (B_tA,))
                    rel(BQ)
                    ts2("dve", tmpA[:], tmpA[:], 1.0, EPS, ALU.mult, ALU.add, (B_tA,), (B_tA,))
                    act(tmpA[:], tmpA[:], AF.Sqrt, (B_tA,), (B_tA,))
                    recip(tmpA[:], tmpA[:], (B_tA,), (B_tA,))
                    gn = gvec(l, 4)
                    for ec in range(2):
                        e = 2 * h + ec
                        tt("dve", tmpB[:], ysb[:, ec, :], mean_sb[:], ALU.subtract, (B_ysb, B_mean), (B_tB,))
                        tt("dve", tmpB[:], tmpB[:], tmpA[:], ALU.mult, (B_tB, B_tA), (B_tB,))
                        stt("dve", yrT[:, e, :], tmpB[:], gn[:, e:e + 1], silu[:, e, :], ALU.mult, ALU.mult, (B_tB, B_silu, B_pvec), (B_yr,))

                if STOP_AFTER <= 4:
                    raise _Stop()
                for m in range(8):
                    w, Bw = next_w()
                    wr = w[:, 0:1024].rearrange("p (k n) -> p k n", k=8)
                    wga = w[:, 1024:2048].rearrange("p (k n) -> p k n", k=8)
                    wgg = w[:, 2048:3072].rearrange("p (k n) -> p k n", k=8)
                    wa = w[:, 3072:3584].rearrange("p (k n) -> p k n", k=4)
                    pGa, BGa = nbank()
                    pGg, BGg = nbank()
                    pA_, BA_ = nbank()
                    pR, BR = nbank()
                    mmgroup([(pGa[:, :], wga[:, k, :], hT[:, k, :], k == 0, k == 7) for k in range(8)], (Bw, B_h), (BGa,))
                    mmgroup([(pGg[:, :], wgg[:, k, :], hT[:, k, :], k == 0, k == 7) for k in range(8)], (Bw, B_h), (BGg,))
                    mmgroup([(pA_[:, :], wa[:, k, :], yaT[:, k, :], k == 0, k == 3) for k in range(4)], (Bw, B_ya), (BA_,))
                    mmgroup([(pR[:, :], wr[:, k, :], yrT[:, k, :], k == 0, k == 7) for k in range(8)], (Bw, B_yr), (BR,))
                    act(gtmp[0][:], pGa[:, :], AF.Sigmoid, (BGa,), (B_gt[0],))
                    act(gtmp[1][:], pGg[:, :], AF.Sigmoid, (BGg,), (B_gt[1],))
                    rel(BGa, BGg)
                    tt("dve", tmpA[:], pA_[:, :], gtmp[0][:], ALU.mult, (BA_, B_gt[0]), (B_tA,))
                    tt("dve", tmpB[:], pR[:, :], gtmp[1][:], ALU.mult, (BR, B_gt[1]), (B_tB,))
                    rel(BA_, BR)
                    tt("pool", merged[:, m, :], tmpA[:], tmpB[:], ALU.add, (B_tA, B_tB), (B_mg, B_vr))
                for mb in range(2):
                    w, Bw = next_w()
                    wv = w[:, 0:4096].rearrange("p (k n) -> p k n", k=8)
                    for jj in range(4):
                        m = mb * 4 + jj
                        pO, BO = nbank()
                        mmgroup([(pO[:, :], wv[:, k, jj * 128:(jj + 1) * 128], merged[:, k, :], k == 0, k == 7) for k in range(8)], (Bw, B_mg), (BO,))
                        act_copy(o_sb[:, m, :], pO[:, :], (BO,), (B_o[m], G_ffn))
                        act(sq[:, m, :], pO[:, :], AF.Square, (BO,), (B_sq[m],))
                        rel(BO)
                postnorm_residual(l, 1)
                if STOP_AFTER <= 5:
                    raise _Stop()

                prenorm(l, 2)
                cw = pvec[:, l * 128 + 40: l * 128 + 128].rearrange("p (f k) -> p f k", k=4)
                for j in range(11):
                    w, Bw = next_w()
                    wv = w[:, 0:4096].rearrange("p (k n) -> p k n", k=8)
                    pa = [proj_fm(wv, Bw, 0), proj_fm(wv, Bw, 1)]
                    pv = [proj_fm(wv, Bw, 2), proj_fm(wv, Bw, 3)]
                    for i in range(2):
                        f = 2 * j + i
                        pa_, Bpa = pa[i]
                        pv_, Bpv = pv[i]
                        ab, Bab = a_sb[i], B_asb[i]
                        ac, Bac = acc[i], B_acc[i]
                        act_copy(ab[:, 2:T + 2], pa_[:, :], (Bpa,), (Bab, G_o))
                        rel(Bpa)
                        cpy("pool", ab[:, 0:2], ccar[:, l * NF + f, :], (B_cc[l],), (Bab,))
                        if t + 1 < n_tiles:
                            cpy("pool", ccar[:, l * NF + f, :], ab[:, T:T + 2], (Bab,), (B_cc[l],))
                        ts2("dve", ac, ab[:, 2:T + 2], cw[:, f, 2:3], cw[:, f, 3:4], ALU.mult, ALU.add, (Bab, B_pvec), (Bac, G_o))
                        stt("dve", ac, ab[:, 1:T + 1], cw[:, f, 1:2], ac, ALU.mult, ALU.add, (Bab, Bac, B_pvec), (Bac,))
                        stt("dve", ac, ab[:, 0:T], cw[:, f, 0:1], ac, ALU.mult, ALU.add, (Bab, Bac, B_pvec), (Bac,))
                        act(gl[i], ac, AF.Gelu_apprx_tanh, (Bac,), (B_gl[i], G_o))
                        tt("dve", gated[:, f, :], gl[i], pv_[:, :], ALU.mult, (B_gl[i], Bpv), G_ar1)
                        rel(Bpv)
                for m in range(8):
                    w, Bw = next_w()
                    wv = w[:, 0:NF * 128].rearrange("p (k n) -> p k n", k=NF)
                    pO, BO = nbank()
                    mmgroup([(pO[:, :], wv[:, k, :], gated[:, k, :], k == 0, k == NF - 1) for k in range(NF)], (Bw, B_gated), (BO,))
                    act_copy(o_sb[:, m, :], pO[:, :], (BO,), (B_o[m], G_ffn))
                    act(sq[:, m, :], pO[:, :], AF.Square, (BO,), (B_sq[m],))
                    rel(BO)
                postnorm_residual(l, 3)
              except _Stop:
                pstate["live"].clear()
            dma("sp", yT_d[:, tsl].rearrange("(c p) t -> p c t", p=128), xT[:], "yo", B_x, ())
        P.ops["sp"].append(([("yo", P.cnt["yo"])], None, None, None))
        P.emit(nc, es)
    return nc


def _prep_shared(inputs, depth, n_tok):
    c = _constants(n_tok)
    f = lambda a: np.ascontiguousarray(np.asarray(a, dtype=np.float32))
    wall = np.stack([_pack_layer(f(inputs["w_in"][l]), f(inputs["w_out_a"][l]), f(inputs["w_out_r"][l]),
                                 f(inputs["w_out"][l]), f(inputs["w_up"][l]), f(inputs["w_down"][l])) for l in range(depth)])
    pv = np.zeros((128, depth * 128), np.float32)
    for l in range(depth):
        def pc(v):
            return f(v).reshape(8, 128).T
        blk = pv[:, l * 128:(l + 1) * 128]
        blk[:, 0:8] = pc(inputs["norm_pre_mix"][l])
        blk[:, 8:16] = pc(inputs["norm_post_mix"][l])
        blk[:, 16:24] = pc(inputs["norm_pre_ffn"][l])
        blk[:, 24:32] = pc(inputs["norm_post_ffn"][l])
        blk[:, 32:40] = pc(inputs["ret_norm"][l])
        cwv = f(inputs["conv_w"][l])
        cbv = f(inputs["conv_b"][l])
        cw4 = np.concatenate([cwv, cbv[None, :]], axis=0)
        blk[:, 40:128] = cw4.reshape(4, NF, 128).transpose(2, 1, 0).reshape(128, NF * 4)
    sinkb = np.ascontiguousarray(np.broadcast_to(f(inputs["sinks"])[:depth].reshape(1, depth * 8), (128, depth * 8)))
    shared = dict(wall=wall, pvec=pv, sinkb=sinkb, relb=f(inputs["rel_bias"]), oh=c["oh"], decay=c["decay"],
                  xib=c["xib"], zeta=c["zeta"], ident=c["ident"], cosT=c["cosT"], sinT=c["sinT"])
    return shared


def kernel(**inputs):
    x = np.asarray(inputs["x"], dtype=np.float32)
    B = x.shape[0]
    shared = _prep_shared(inputs, DEPTH, SEQ)
    nc = build(SEQ // T, DEPTH)
    in_maps = []
    for b in range(B):
        m = dict(shared)
        m["xT"] = np.ascontiguousarray(x[b].T)
        in_maps.append(m)
    res = run_bass_kernel_spmd(nc, in_maps, core_ids=list(range(B)))
    out = np.stack([np.ascontiguousarray(np.asarray(r["yT"], dtype=np.float32).T) for r in res.results], axis=0)
    return out.astype(np.float32)
```

```python
import math
from contextlib import ExitStack
import numpy as np
import concourse.bass as bass
import concourse.mybir as mybir
from concourse.bass_utils import run_bass_kernel_spmd

F32, BF16 = mybir.dt.float32, mybir.dt.bfloat16
ALU, AF = mybir.AluOpType, mybir.ActivationFunctionType

D = 1024
SEQ = 4096
DEPTH = 4
T = 512
NB = 4
A_HEADS, A_KV, A_HD = 8, 2, 64
R_HEADS, R_DK, R_DV = 4, 128, 256
D_FF = 2816
NF = 22
NUM_BUCKETS, MAX_DISTANCE = 32, 128
EPS = 1e-6
NEG = -30000.0
SEM_LIMIT = 30000
NWS = 4
DEBUG_TAGS = None
STOP_AFTER = 99


class _Stop(Exception):
    pass

WSLOT = 4096

W1_GROUPS = [
    [("qa", 0), ("qa", 1), ("qa", 2), ("qa", 3)],
    [("ka", 0), ("va", 0), ("vr", 0), ("vr", 1)],
    [("vr", 2), ("vr", 3), ("vr", 4), ("vr", 5)],
    [("vr", 6), ("vr", 7), ("gr", 0), ("gr", 1)],
    [("qr", 0), ("qrs", 0), ("kr", 0), ("krs", 0)],
    [("qr", 1), ("qrs", 1), ("kr", 1), ("krs", 1)],
    [("qr", 2), ("qrs", 2), ("kr", 2), ("krs", 2)],
    [("qr", 3), ("qrs", 3), ("kr", 3), ("krs", 3)],
    [("gr", 2), ("gr", 3), ("gr", 4), ("gr", 5)],
    [("gr", 6), ("gr", 7)],
]


def _layer_groups():
    gs = []
    for roles in W1_GROUPS:
        gs.append(("w1", roles, 8 * 128 * len(roles)))
    for m in range(8):
        gs.append(("m", m, 8 * 128 * 3 + 4 * 128))
    for mb in range(2):
        gs.append(("wo", mb, 4096))
    for j in range(11):
        gs.append(("up", j, 4096))
    for m in range(8):
        gs.append(("dn", m, NF * 128))
    return gs


LAYER_GROUPS = _layer_groups()
WTOT = sum(g[2] for g in LAYER_GROUPS)


def _w1_cols(role, idx):
    A_Q, A_K = 512, 128
    o_qa, o_ka, o_va = 0, 512, 640
    o_qr, o_kr, o_vr, o_gr = 768, 1280, 1792, 2816
    ar = np.arange
    if role == "qa":
        return np.concatenate([o_qa + idx * 64 + ar(64), o_qa + (4 + idx) * 64 + ar(64)])
    if role == "ka":
        return o_ka + ar(128)
    if role == "va":
        return o_va + ar(128)
    if role == "qr":
        return o_qr + idx * 128 + ar(128)
    if role == "qrs":
        return o_qr + idx * 128 + np.concatenate([64 + ar(64), ar(64)])
    if role == "kr":
        return o_kr + idx * 128 + ar(128)
    if role == "krs":
        return o_kr + idx * 128 + np.concatenate([64 + ar(64), ar(64)])
    if role == "vr":
        return o_vr + idx * 128 + ar(128)
    if role == "gr":
        return o_gr + idx * 128 + ar(128)
    raise ValueError(role)


def _kchunks(w):
    K, n = w.shape
    return np.ascontiguousarray(w.reshape(K // 128, 128, n).transpose(1, 0, 2)).reshape(128, -1)


def _pack_layer(w_in, w_out_a, w_out_r, w_out, w_up, w_down):
    o_ga, o_gg = 3840, 4864
    parts = []
    rows_a = np.concatenate([np.concatenate([c * 64 + np.arange(64), (4 + c) * 64 + np.arange(64)]) for c in range(4)])
    w_oa = w_out_a[rows_a]
    for kind, arg, size in LAYER_GROUPS:
        if kind == "w1":
            cols = np.concatenate([_w1_cols(r, i) for r, i in arg])
            parts.append(_kchunks(w_in[:, cols]))
        elif kind == "m":
            m = arg
            cs = slice(m * 128, (m + 1) * 128)
            parts.append(np.concatenate([
                _kchunks(w_out_r[:, cs]),
                _kchunks(w_in[:, o_ga + m * 128:o_ga + (m + 1) * 128]),
                _kchunks(w_in[:, o_gg + m * 128:o_gg + (m + 1) * 128]),
                _kchunks(w_oa[:, cs]),
            ], axis=1))
        elif kind == "wo":
            parts.append(_kchunks(w_out[:, arg * 512:(arg + 1) * 512]))
        elif kind == "up":
            j = arg
            cols = np.concatenate([np.arange(2 * j * 128, (2 * j + 2) * 128), D_FF + np.arange(2 * j * 128, (2 * j + 2) * 128)])
            parts.append(_kchunks(w_up[:, cols]))
        elif kind == "dn":
            parts.append(_kchunks(w_down[:, arg * 128:(arg + 1) * 128]))
    out = np.concatenate(parts, axis=1)
    assert out.shape == (128, WTOT), out.shape
    return out


def _t5_bucket(n):
    max_exact = NUM_BUCKETS // 2
    n = np.maximum(n, 0)
    large = max_exact + (np.log(np.maximum(n, 1) / max_exact) / math.log(MAX_DISTANCE / max_exact)
                         * (NUM_BUCKETS - max_exact)).astype(np.int32)
    large = np.minimum(large, NUM_BUCKETS - 1)
    return np.where(n < max_exact, n, large).astype(np.int32)


def _constants(n_tok):
    c = {}
    oh = np.zeros((33, 2 * 255), np.float32)
    for pc in range(2):
        for i in range(255):
            dist = (127 - i) if pc == 1 else (255 - i)
            valid = (dist >= 0) and (dist < 128)
            b = int(_t5_bucket(np.array([dist]))[0]) if valid else 32
            oh[b, pc * 255 + i] = 1.0
    c["oh"] = oh
    lg = np.log(1.0 - 2.0 ** (-5.0 - np.arange(R_HEADS, dtype=np.float64)))
    idx = np.arange(128, dtype=np.float64)
    sc = R_DK ** -0.5
    rel = idx[None, :] - idx[:, None]
    dec = np.where(rel >= 0, np.exp(lg[:, None, None] * np.maximum(rel, 0.0)), 0.0) * sc
    c["decay"] = np.ascontiguousarray(dec.transpose(1, 0, 2)).reshape(128, 512).astype(np.float32)
    xi = np.exp(lg[:, None] * (idx + 1.0)[None, :])
    c["xib"] = np.ascontiguousarray(np.broadcast_to(xi.reshape(1, 512), (128, 512))).astype(np.float32)
    zeta = np.exp(lg[:, None] * (127 - idx)[None, :]) * sc
    c["zeta"] = np.ascontiguousarray(zeta.T).astype(np.float32)
    c["ident"] = np.eye(128, dtype=np.float32)
    half = 64
    freqs = (10000.0 ** (-np.arange(half, dtype=np.float32) / half)).astype(np.float32)
    ang = (np.arange(n_tok, dtype=np.float32)[None, :] * freqs[:, None]).astype(np.float32)
    cs, sn = np.cos(ang.astype(np.float64)), np.sin(ang.astype(np.float64))
    c["cosT"] = np.concatenate([cs, cs], axis=0).astype(np.float32)
    c["sinT"] = np.concatenate([-sn, sn], axis=0).astype(np.float32)
    return c


GAMMA_C = [float((1.0 - 2.0 ** (-5.0 - h)) ** 128) for h in range(R_HEADS)]


class Buf:
    __slots__ = ("name", "w", "r")

    def __init__(self, name):
        self.name = name
        self.w = None
        self.r = {}


class Prog:
    ENG = ("pe", "act", "dve", "pool", "sp")
    BLK = {"pe": "tensor", "act": "scalar", "dve": "vector", "pool": "gpsimd", "sp": "sync"}

    def __init__(self):
        self.ops = {e: [] for e in self.ENG}
        self.cnt = {}
        self.seen = {e: {} for e in self.ENG}
        self.epoch = {e: 0 for e in self.ENG}
        self.debug_tags = None

    def _need(self, e, ts, waits):
        if ts is None:
            return
        k, v = ts
        if self.seen[e].get(k, 0) >= v:
            return
        assert self.cnt.get(k, 0) >= v, ("unresolved timestamp", k, v)
        if e == "pe" and isinstance(k, tuple) and k[0] == "pe":
            return
        waits[k] = max(waits.get(k, 0), v)

    @staticmethod
    def _flat(x):
        out = []
        for b in x:
            if isinstance(b, Buf):
                out.append(b)
            else:
                out.extend(Prog._flat(b))
        return out

    def _waits(self, e, reads, writes):
        waits = {}
        for b in reads:
            self._need(e, b.w, waits)
        for b in writes:
            self._need(e, b.w, waits)
            for k, v in b.r.items():
                self._need(e, (k, v), waits)
        for k, v in waits.items():
            self.seen[e][k] = v
        return list(waits.items())

    def _mark(self, ts, reads, writes):
        k, v = ts
        for b in reads:
            if b.r.get(k, 0) < v:
                b.r[k] = v
        for b in writes:
            b.w = ts
            b.r = {}

    def op(self, e, fn, reads=(), writes=()):
        reads, writes = self._flat(reads), self._flat(writes)
        waits = self._waits(e, reads, writes)
        k = (e, self.epoch[e])
        self.cnt[k] = self.cnt.get(k, 0) + 1
        ts = (k, self.cnt[k])
        if self.cnt[k] >= SEM_LIMIT:
            self.epoch[e] += 1
        import sys as _s
        fr = _s._getframe(1)
        tags = []
        while fr is not None and len(tags) < 3:
            tags.append(fr.f_lineno)
            fr = fr.f_back
        self.ops[e].append((waits, fn, (k, 1), tags))
        self._mark(ts, reads, writes)

    def dma(self, q, fn, semkey, reads=(), writes=()):
        reads, writes = self._flat(reads), self._flat(writes)
        waits = self._waits(q, reads, writes)
        self.cnt[semkey] = self.cnt.get(semkey, 0) + 16
        ts = (semkey, self.cnt[semkey])
        self.ops[q].append((waits, fn, (semkey, 16), None))
        self._mark(ts, reads, writes)

    def wait_all(self, e, bufs):
        waits = self._waits(e, bufs, ())
        self.ops[e].append((waits, None, None, None))

    def emit(self, nc, es):
        keys = list(self.cnt.keys())
        sems = {}
        for i, k in enumerate(keys):
            sems[k] = es.enter_context(nc.semaphore("s%d" % i))
        block = es.enter_context(nc.Block())
        for e in self.ENG:
            oplist = self.ops[e]

            def body(eng, oplist=oplist):
                for waits, fn, inc, tags in oplist:
                    for k, v in waits:
                        eng.wait_ge(sems[k], v)
                    if fn is None:
                        continue
                    ins = fn(eng)
                    ins.then_inc(sems[inc[0]], inc[1])
                    if self.debug_tags is not None:
                        try:
                            self.debug_tags[str(ins.ins.name)] = tags
                        except Exception:
                            pass

            getattr(block, self.BLK[e])(body)


def build(n_tiles=8, depth=DEPTH):
    n_tok = n_tiles * T
    nc = bass.Bass("TRN2", target_bir_lowering=False)
    dr = {}

    def din(name, shape):
        dr[name] = nc.dram_tensor(name, list(shape), F32, kind="ExternalInput").ap()
        return dr[name]

    xT_d = din("xT", [D, n_tok])
    wall_d = din("wall", [depth, 128, WTOT])
    pvec_d = din("pvec", [128, depth * 128])
    sinkb_d = din("sinkb", [128, depth * 8])
    relb_d = din("relb", [32, 8])
    oh_d = din("oh", [33, 510])
    decay_d = din("decay", [128, 512])
    xib_d = din("xib", [128, 512])
    zeta_d = din("zeta", [128, 4])
    ident_d = din("ident", [128, 128])
    cos_d = din("cosT", [128, n_tok])
    sin_d = din("sinT", [128, n_tok])
    yT_d = nc.dram_tensor("yT", [D, n_tok], F32, kind="ExternalOutput").ap()

    P = Prog()
    if DEBUG_TAGS is not None:
        P.debug_tags = DEBUG_TAGS
    es = ExitStack()
    with es:
        def sb(name, shape, dt):
            return es.enter_context(nc.sbuf_tensor("s_" + name, list(shape), dt))

        def ps(name, shape, dt):
            return es.enter_context(nc.psum_tensor("p_" + name, list(shape), dt))

        xT = sb("xT", [128, 8, T], F32); B_x = [Buf("xT%d" % c) for c in range(8)]
        S32 = sb("S32", [128, depth * 4, 256], F32); B_S32 = [[Buf("S32") for _ in range(4)] for _ in range(depth)]
        Sbf = sb("Sbf", [128, 4, 256], BF16); B_Sbf = [Buf("Sbf") for _ in range(4)]
        kaT = sb("kaT", [128, depth, 640], BF16); B_ka = [Buf("ka") for _ in range(depth)]
        vaT = sb("vaT", [128, depth * 5, 128], BF16); B_va = [Buf("va") for _ in range(depth)]
        ccar = sb("ccar", [128, depth * NF, 2], F32); B_cc = [Buf("cc") for _ in range(depth)]
        bias = sb("bias", [128, 2 * 8, 128], F32); B_bias = Buf("bias")
        decay = sb("decay", [128, 512], F32); B_decay = Buf("decay")
        xib = sb("xib", [128, 512], F32); B_xib = Buf("xib")
        zeta = sb("zeta", [128, 4], F32); B_zeta = Buf("zeta")
        pvec = sb("pvec", [128, depth * 128], F32); B_pvec = Buf("pvec")
        esink = sb("esink", [128, depth * 8], F32); B_es = Buf("esink")
        identb = sb("identb", [128, 128], BF16); B_ident = Buf("ident")
        onesb = sb("onesb", [128, 128], BF16); B_ones = Buf("ones")
        inv256 = sb("inv256", [128, 128], BF16)
        cosS = sb("cosS", [128, T], F32); B_cos = Buf("cos")
        sinS = sb("sinS", [128, T], F32); B_sin = Buf("sin")
        wsl = [sb("ws%d" % i, [128, WSLOT], BF16) for i in range(NWS)]
        B_ws = [Buf("ws%d" % i) for i in range(NWS)]
        hT = sb("hT", [128, 8, T], BF16); B_h = Buf("hT")
        ar3 = sb("ar3", [128, 8 * T], BF16)
        sq = ar3[:, :].rearrange("p (c t) -> p c t", c=8)
        qaT = ar3[:, 0:4 * T].rearrange("p (c t) -> p c t", c=4)
        qrT = ar3[:, 4 * T:8 * T].rearrange("p (c t) -> p c t", c=4)
        B_sq = [Buf("sq%d" % c) for c in range(8)]
        B_qa = B_sq[0:4]
        B_qr = B_sq[4:8]
        ar1 = sb("ar1", [128, NF * T], BF16)
        gated = ar1[:, :].rearrange("p (f t) -> p f t", f=NF)
        q2T = ar1[:, 0:2048].rearrange("p (c t) -> p c t", c=4)
        krT = ar1[:, 2048:4096].rearrange("p (c t) -> p c t", c=4)
        PTa = [ar1[:, 4096 + i * 1024: 4096 + (i + 1) * 1024].rearrange("p (c t) -> p c t", c=2) for i in range(2)]
        PTr = ar1[:, 6144:6656]
        Kz = ar1[:, 6656:7168].rearrange("p (n d) -> p n d", n=4)
        yaT = ar1[:, 7168:9216].rearrange("p (c t) -> p c t", c=4)
        ysb = ar1[:, 9216:10240].rearrange("p (c t) -> p c t", c=2)
        ysq = ar1[:, 10240:11264].rearrange("p (c t) -> p c t", c=2)
        B_gated = Buf("gated"); B_q2 = Buf("q2"); B_kr = Buf("kr"); B_PTa = [Buf("PTa0"), Buf("PTa1")]
        B_PTr = Buf("PTr"); B_Kz = Buf("Kz"); B_ya = Buf("ya"); B_ysb = Buf("ysb"); B_ysq = Buf("ysq")
        G_ar1 = (B_gated, B_q2, B_kr, B_PTa[0], B_PTa[1], B_PTr, B_Kz, B_ya, B_ysb, B_ysq)
        tmpA = sb("tmpA", [128, T], F32); B_tA = Buf("tmpA")
        tmpB = sb("tmpB", [128, T], F32); B_tB = Buf("tmpB")
        tmpC = sb("tmpC", [128, T], BF16); B_tC = Buf("tmpC")
        tmpD = sb("tmpD", [128, T], BF16); B_tD = Buf("tmpD")
        tmpE = sb("tmpE", [128, T], F32); B_tE = Buf("tmpE")
        tmpF = sb("tmpF", [128, T], F32); B_tF = Buf("tmpF")
        PTrX = sb("PTrX", [128, 3, T], BF16)
        KzX = sb("KzX", [128, 3 * 4, 128], BF16)
        ysb2 = sb("ysb2", [128, 2, T], BF16)
        ysq2 = sb("ysq2", [128, 2, T], BF16)
        ysqt = sb("ysqt", [128, 2, T], BF16); B_ysqt = Buf("ysqt")
        arV = sb("arV", [128, 4096], BF16)
        vr = arV[:, :].rearrange("p (n e) -> p n e", n=4)
        merged = arV[:, :].rearrange("p (c t) -> p c t", c=8)
        B_vr = Buf("vr"); B_mg = Buf("merged")
        silu = sb("silu", [128, 8, T], BF16); B_silu = Buf("silu")
        yrT = sb("yrT", [128, 8, T], BF16); B_yr = Buf("yr")
        gtmp = [sb("gtmp%d" % i, [128, T], BF16) for i in range(2)]; B_gt = [Buf("gt%d" % i) for i in range(2)]
        arF = sb("arF", [128, 8 * T], F32)
        o_sb = arF[:, :].rearrange("p (c t) -> p c t", c=8)
        a_sb = [arF[:, i * 516: i * 516 + T + 2] for i in range(2)]
        acc = [arF[:, 1032 + i * T: 1032 + (i + 1) * T] for i in range(2)]
        gl = [arF[:, 2056 + i * T: 2056 + (i + 1) * T] for i in range(2)]
        B_o = [Buf("o%d" % c) for c in range(8)]
        B_asb = [Buf("asb%d" % i) for i in range(2)]; B_acc = [Buf("acc%d" % i) for i in range(2)]; B_gl = [Buf("gl%d" % i) for i in range(2)]
        G_ffn = tuple(B_asb + B_acc + B_gl)
        G_o = tuple(B_o)
        PTrs = [PTr] + [PTrX[:, i, :] for i in range(3)]; B_PTrs = [(B_PTr, B_gated)] + [(Buf("PTrX%d" % i),) for i in range(3)]
        Kzs = [Kz] + [KzX[:, 4 * i:4 * i + 4, :] for i in range(3)]; B_Kzs = [(B_Kz, B_gated)] + [(Buf("KzX%d" % i),) for i in range(3)]
        ysbs = [ysb, ysq, ysb2[:, :, :], ysq2[:, :, :]]; B_ysbs = [(B_ysb, B_gated), (B_ysq, B_gated), (Buf("ysb2"),), (Buf("ysq2"),)]
        rstd = sb("rstd", [128, T], F32); B_rstd = Buf("rstd")
        mean_sb = sb("mean_sb", [128, T], F32); B_mean = Buf("mean")
        tab = sb("tab", [33, 8], F32)
        tabh = sb("tabh", [33, 8], BF16)
        tabh32 = sb("tabh32", [33, 8], F32)
        tabl = sb("tabl", [33, 8], BF16)
        ohb = sb("ohb", [33, 510], BF16); B_oh = Buf("oh")
        B_tab = Buf("tab")
        NPB = 7
        pbank = [ps("pb%d" % i, [128, 512], F32) for i in range(NPB)]
        B_pb = [Buf("pb%d" % i) for i in range(NPB)]
        ptr = ps("ptr", [128, 1024], BF16); B_ptr = Buf("ptr")
        pstate = {"i": 0, "live": set()}

        def nbank():
            for d_ in range(NPB):
                i = (pstate["i"] + d_) % NPB
                if i not in pstate["live"]:
                    pstate["i"] = (i + 1) % NPB
                    pstate["live"].add(i)
                    return pbank[i], B_pb[i]
            raise RuntimeError("no free PSUM bank")

        def rel(*bs):
            for b_ in bs:
                pstate["live"].discard(B_pb.index(b_))

        def mmgroup(specs, reads, writes):
            def fn(eng, specs=specs):
                ins = None
                for (o, l_, r_, st, sp_) in specs:
                    ins = eng.matmul(o, lhsT=l_, rhs=r_, start=st, stop=sp_)
                return ins
            P.op("pe", fn, reads, writes)

        def transposes(specs, reads, writes):
            def fn(eng, specs=specs):
                ins = None
                for (o, i_) in specs:
                    ins = eng.transpose(out=o, in_=i_, identity=identb[:])
                return ins
            P.op("pe", fn, reads, writes)

        def act(out, in_, func, reads, writes, scale=None):
            def fn(eng):
                if scale is None:
                    return eng.activation(out=out, in_=in_, func=func)
                return eng.activation(out=out, in_=in_, func=func, scale=scale)
            P.op("act", fn, reads, writes)

        def act_mul(out, in_, mul, reads, writes):
            P.op("act", lambda eng: eng.mul(out=out, in_=in_, mul=mul), reads, writes)

        def act_copy(out, in_, reads, writes):
            P.op("act", lambda eng: eng.copy(out=out, in_=in_), reads, writes)

        def tt(e, out, in0, in1, op, reads, writes):
            P.op(e, lambda eng: eng.tensor_tensor(out=out, in0=in0, in1=in1, op=op), reads, writes)

        def ts2(e, out, in0, s1, s2, op0, op1, reads, writes):
            P.op(e, lambda eng: eng.tensor_scalar(out=out, in0=in0, scalar1=s1, scalar2=s2, op0=op0, op1=op1), reads, writes)

        def stt(e, out, in0, scalar, in1, op0, op1, reads, writes):
            P.op(e, lambda eng: eng.scalar_tensor_tensor(out=out, in0=in0, scalar=scalar, in1=in1, op0=op0, op1=op1), reads, writes)

        def cpy(e, out, in_, reads, writes):
            P.op(e, lambda eng: eng.tensor_copy(out=out, in_=in_), reads, writes)

        def recip(out, in_, reads, writes):
            P.op("dve", lambda eng: eng.reciprocal(out=out, in_=in_), reads, writes)

        def memset(e, ap, val, writes):
            P.op(e, lambda eng: eng.memset(ap, val), (), writes)

        def dma(q, out, in_, semkey, reads, writes):
            P.dma(q, lambda eng: eng.dma_start(out=out, in_=in_), semkey, reads, writes)

        gseq = []
        for t in range(n_tiles):
            for l in range(depth):
                off = 0
                for (kind, arg, size) in LAYER_GROUPS:
                    gseq.append((l, off, size))
                    off += size
        wstate = {"issued": 0, "used": 0}

        def issue_w():
            g = wstate["issued"]
            if g >= len(gseq):
                return
            l_, off, size = gseq[g]
            s_ = g % NWS
            dma("pool", wsl[s_][:, 0:size], wall_d[l_, :, off:off + size], "w%d" % s_, (), (B_ws[s_],))
            wstate["issued"] = g + 1

        def next_w():
            g = wstate["used"]
            wstate["used"] = g + 1
            while wstate["issued"] < min(g + NWS, len(gseq)):
                issue_w()
            s_ = g % NWS
            return wsl[s_], B_ws[s_]

        dma("sp", decay[:], decay_d, "su0", (), (B_decay,))
        dma("sp", xib[:], xib_d, "su1", (), (B_xib,))
        dma("sp", zeta[:], zeta_d, "su2", (), (B_zeta,))
        dma("sp", pvec[:], pvec_d, "su3", (), (B_pvec,))
        dma("sp", esink[:], sinkb_d, "su4", (), (B_es,))
        dma("sp", tab[0:32, :], relb_d, "su5", (), (B_tab,))
        dma("pool", identb[:], ident_d, "su6", (), (B_ident,))
        dma("pool", ohb[:], oh_d, "su7", (), (B_oh,))
        for _ in range(NWS - 1):
            issue_w()
        memset("dve", onesb[:], 1.0, (B_ones,))
        memset("dve", inv256[:], 1.0 / 256.0, (B_ones,))
        memset("dve", S32[:], 0.0, B_S32)
        memset("dve", kaT[:], 0.0, B_ka)
        memset("dve", vaT[:], 0.0, B_va)
        memset("dve", ccar[:], 0.0, B_cc)
        memset("dve", tab[32:33, :], NEG, (B_tab,))
        act(esink[:], esink[:], AF.Exp, (B_es,), (B_es,))
        cpy("dve", tabh[:], tab[:], (B_tab,), (B_tab,))
        cpy("dve", tabh32[:], tabh[:], (B_tab,), (B_tab,))
        tt("dve", tabh32[:], tab[:], tabh32[:], ALU.subtract, (B_tab,), (B_tab,))
        cpy("dve", tabl[:], tabh32[:], (B_tab,), (B_tab,))
        for b4 in range(4):
            pc, q0 = b4 // 2, (b4 % 2) * 64
            pb_, Bp = nbank()
            specs = []
            for qq in range(64):
                q = q0 + qq
                lh = ohb[:, pc * 255 + 127 - q: pc * 255 + 255 - q]
                specs.append((pb_[:, qq * 8:(qq + 1) * 8], lh, tabh[:], True, False))
                specs.append((pb_[:, qq * 8:(qq + 1) * 8], lh, tabl[:], False, True))
            mmgroup(specs, (B_tab, B_oh), (Bp,))
            cpy("dve", bias[:, pc * 8:(pc + 1) * 8, q0:q0 + 64], pb_[:, :].rearrange("p (q h) -> p h q", h=8), (Bp,), (B_bias,))
            rel(Bp)

        def gvec(l, j):
            return pvec[:, l * 128 + j * 8: l * 128 + (j + 1) * 8]

        def rms_rstd():
            pb_, Bp = nbank()
            specs = [(pb_[:, :], onesb[:], sq[:, c, :], c == 0, c == 7) for c in range(8)]
            mmgroup(specs, (B_sq, B_ones), (Bp,))
            ts2("dve", rstd[:], pb_[:, :], 1.0 / D, EPS, ALU.mult, ALU.add, (Bp,), (B_rstd,))
            rel(Bp)
            act(rstd[:], rstd[:], AF.Sqrt, (B_rstd,), (B_rstd,))
            recip(rstd[:], rstd[:], (B_rstd,), (B_rstd,))

        def prenorm(l, j):
            for c in range(8):
                act(sq[:, c, :], xT[:, c, :], AF.Square, (B_x[c],), (B_sq[c],))
            rms_rstd()
            g = gvec(l, j)
            for c in range(8):
                stt("dve", hT[:, c, :], xT[:, c, :], g[:, c:c + 1], rstd[:], ALU.mult, ALU.mult, (B_x[c], B_rstd, B_pvec), (B_h,))

        def postnorm_residual(l, j):
            rms_rstd()
            g = gvec(l, j)
            for c in range(8):
                stt("dve", o_sb[:, c, :], o_sb[:, c, :], g[:, c:c + 1], rstd[:], ALU.mult, ALU.mult, (B_o[c], B_rstd, B_pvec), (B_o[c],))
                tt("pool", xT[:, c, :], xT[:, c, :], o_sb[:, c, :], ALU.add, (B_x[c], B_o[c]), (B_x[c],))

        def proj_fm(wv, Bw, j):
            pb_, Bp = nbank()
            specs = [(pb_[:, :], wv[:, k, j * 128:(j + 1) * 128], hT[:, k, :], k == 0, k == 7) for k in range(8)]
            mmgroup(specs, (Bw, B_h), (Bp,))
            return pb_, Bp

        def proj_tm(wv, Bw, j):
            pb_, Bp = nbank()
            specs = []
            for n in range(NB):
                for k in range(8):
                    specs.append((pb_[:, n * 128:(n + 1) * 128], hT[:, k, n * 128:(n + 1) * 128], wv[:, k, j * 128:(j + 1) * 128], k == 0, k == 7))
            mmgroup(specs, (Bw, B_h), (Bp,))
            return pb_, Bp

        def do_w1_group(l, roles):
            w, Bw = next_w()
            nch = len(roles)
            wv = w[:, 0:8 * 128 * nch].rearrange("p (k n) -> p k n", k=8)
            j = 0
            while j < nch:
                role, idx = roles[j]
                if role == "qa":
                    pb_, Bp = proj_fm(wv, Bw, j)
                    act_mul(qaT[:, idx, :], pb_[:, :], A_HD ** -0.5, (Bp,), (B_qa[idx],))
                    rel(Bp)
                elif role == "ka":
                    pb_, Bp = proj_fm(wv, Bw, j)
                    act_copy(kaT[:, l, 128:640], pb_[:, :], (Bp,), (B_ka[l],))
                    rel(Bp)
                elif role == "va":
                    pb_, Bp = proj_tm(wv, Bw, j)
                    act_copy(vaT[:, l * 5 + 1:l * 5 + 5, :], pb_[:, :].rearrange("p (n c) -> p n c", n=4), (Bp,), (B_va[l],))
                    rel(Bp)
                elif role == "vr":
                    pb_, Bp = proj_tm(wv, Bw, j)
                    act_copy(vr[:, :, idx * 128:(idx + 1) * 128], pb_[:, :].rearrange("p (n c) -> p n c", n=4), (Bp,), (B_vr, B_mg))
                    rel(Bp)
                elif role == "gr":
                    pb_, Bp = proj_fm(wv, Bw, j)
                    act(silu[:, idx, :], pb_[:, :], AF.Silu, (Bp,), (B_silu,))
                    rel(Bp)
                elif role in ("qr", "kr"):
                    pq, Bq = proj_fm(wv, Bw, j)
                    pq2, Bq2 = proj_fm(wv, Bw, j + 1)
                    tt("dve", tmpC[:], pq[:, :], cosS[:], ALU.mult, (Bq, B_cos), (B_tC,))
                    tt("dve", tmpD[:], pq2[:, :], sinS[:], ALU.mult, (Bq2, B_sin), (B_tD,))
                    rel(Bq, Bq2)
                    if role == "qr":
                        tt("pool", qrT[:, idx, :], tmpC[:], tmpD[:], ALU.add, (B_tC, B_tD), (B_qr[idx],))
                        tt("pool", q2T[:, idx, :].rearrange("p (n c) -> p n c", n=4), qrT[:, idx, :].rearrange("p (n c) -> p n c", n=4),
                           bass.AP(xib, idx * 128, [[512, 128], [0, 4], [1, 128]]), ALU.mult, (B_qr[idx], B_xib), (B_q2, B_gated))
                    else:
                        tt("pool", krT[:, idx, :], tmpC[:], tmpD[:], ALU.add, (B_tC, B_tD), (B_kr, B_gated))
                    j += 1
                else:
                    raise ValueError(role)
                j += 1

        for t in range(n_tiles):
            tsl = slice(t * T, (t + 1) * T)
            dma("sp", xT[:], xT_d[:, tsl].rearrange("(c p) t -> p c t", p=128), "xl", (), B_x)
            dma("sp", cosS[:], cos_d[:, tsl], "cl", (), (B_cos,))
            dma("sp", sinS[:], sin_d[:, tsl], "sl", (), (B_sin,))
            for l in range(depth):
              try:
                first_blk = (t == 0)
                if STOP_AFTER <= 0:
                    raise _Stop()
                prenorm(l, 0)
                if STOP_AFTER <= 1:
                    raise _Stop()
                for gi in range(4):
                    do_w1_group(l, W1_GROUPS[gi])
                fillers = [(lambda gi=gi, l=l: do_w1_group(l, W1_GROUPS[gi])) for gi in range(4, len(W1_GROUPS))]
                if STOP_AFTER <= 2:
                    raise _Stop()

                pairs = [(n, j) for n in range(NB) for j in range(A_KV)]

                def att_scores(i):
                    n, j = pairs[i]
                    prs = slice(j * 64, (j + 1) * 64)
                    out = []
                    for pc in range(2):
                        if pc == 0 and first_blk and n == 0:
                            out.append(None)
                            continue
                        pb_, Bp = nbank()
                        kc = slice((n + pc) * 128, (n + pc + 1) * 128)
                        specs = [(pb_[:, :], kaT[prs, l, kc], qaT[prs, :, n * 128:(n + 1) * 128], True, True)]
                        mmgroup(specs, (B_ka[l], B_qa), (Bp,))
                        out.append((pb_, Bp))
                    return out

                def att_rest(i, sc):
                    n, j = pairs[i]
                    prs = slice(j * 64, (j + 1) * 64)
                    pt, Bpt = PTa[i % 2], B_PTa[i % 2]
                    used = []
                    for pc in range(2):
                        if sc[pc] is None:
                            continue
                        pb_, Bp = sc[pc]
                        bview = bias[:, pc * 8 + j * 4: pc * 8 + j * 4 + 4, :]
                        tt("dve", tmpA[:].rearrange("p (c q) -> p c q", c=4), pb_[:, :].rearrange("p (c q) -> p c q", c=4), bview, ALU.add, (Bp, B_bias), (B_tA,))
                        rel(Bp)
                        act(pt[:, pc, :], tmpA[:], AF.Exp, (B_tA,), (Bpt, B_gated))
                        used.append(pc)
                    pn, Bn = nbank()
                    pd, Bd = nbank()
                    specs = []
                    for ii, pc in enumerate(used):
                        specs.append((pn[:, :], vaT[:, l * 5 + n + pc, :], pt[:, pc, :], ii == 0, ii == len(used) - 1))
                    for ii, pc in enumerate(used):
                        specs.append((pd[:, :], onesb[:], pt[:, pc, :], ii == 0, ii == len(used) - 1))
                    mmgroup(specs, (B_va[l], Bpt, B_ones), (Bn, Bd))
                    esv = bass.AP(esink, l * 8 + j * 4, [[depth * 8, 128], [1, 4], [0, 128]])
                    tt("dve", tmpB[:].rearrange("p (c q) -> p c q", c=4), pd[:, :].rearrange("p (c q) -> p c q", c=4), esv, ALU.add, (Bd, B_es), (B_tB,))
                    rel(Bd)
                    recip(tmpB[:], tmpB[:], (B_tB,), (B_tB,))
                    tt("dve", yaT[prs, :, n * 128:(n + 1) * 128], pn[prs, :].rearrange("p (c q) -> p c q", c=4),
                       tmpB[prs, :].rearrange("p (c q) -> p c q", c=4), ALU.mult, (Bn, B_tB), (B_ya, B_gated))
                    rel(Bn)

                def ret_setup(h):
                    PTr_, BPTr_, Kz_, BKz_ = PTrs[h], B_PTrs[h], Kzs[h], B_Kzs[h]
                    pA, BA = nbank()
                    specs = [(pA[:, n * 128:(n + 1) * 128], krT[:, h, n * 128:(n + 1) * 128], qrT[:, h, n * 128:(n + 1) * 128], True, True) for n in range(NB)]
                    mmgroup(specs, (B_kr, B_qr[h]), (BA,))
                    tt("dve", PTr_.rearrange("p (n c) -> p n c", n=4), pA[:, :].rearrange("p (n c) -> p n c", n=4),
                       bass.AP(decay, h * 128, [[512, 128], [0, 4], [1, 128]]), ALU.mult, (BA, B_decay), (BPTr_,))
                    rel(BA)
                    transposes([(ptr[:, n * 128:(n + 1) * 128], krT[:, h, n * 128:(n + 1) * 128]) for n in range(NB)], (B_kr, B_ident), (B_ptr,))
                    act_mul(Kz_.rearrange("p n d -> p (n d)"), ptr[:, 0:512], zeta[:, h:h + 1], (B_ptr, B_zeta), (BKz_,))

                def ret_chunk(n, h):
                    PTr_, BPTr_, Kz_, BKz_ = PTrs[h], B_PTrs[h], Kzs[h], B_Kzs[h]
                    ysb_, Bysb_ = ysbs[h], B_ysbs[h]
                    act_copy(Sbf[:, h, :], S32[:, l * 4 + h, :], (B_S32[l][h],), (B_Sbf[h],))
                    pY, BY = nbank()
                    specs = []
                    for ec in range(2):
                        o = pY[:, ec * 128:(ec + 1) * 128]
                        specs.append((o, vr[:, n, h * 256 + ec * 128: h * 256 + (ec + 1) * 128], PTr_[:, n * 128:(n + 1) * 128], True, False))
                        specs.append((o, Sbf[:, h, ec * 128:(ec + 1) * 128], q2T[:, h, n * 128:(n + 1) * 128], False, True))
                    mmgroup(specs, (B_vr, BPTr_, B_Sbf[h], B_q2), (BY,))
                    pK, BK = nbank()
                    mmgroup([(pK[:, 0:256], Kz_[:, n, :], vr[:, n, h * 256:(h + 1) * 256], True, True)], (BKz_, B_vr), (BK,))
                    stt("dve", S32[:, l * 4 + h, :], S32[:, l * 4 + h, :], GAMMA_C[h], pK[:, 0:256], ALU.mult, ALU.add, (B_S32[l][h], BK), (B_S32[l][h],))
                    rel(BK)
                    act_copy(ysb_[:, :, n * 128:(n + 1) * 128], pY[:, 0:256].rearrange("p (e c) -> p e c", e=2), (BY,), (Bysb_,))
                    rel(BY)

                def ret_norm(h):
                    ysb_, Bysb_ = ysbs[h], B_ysbs[h]
                    act(ysqt[:], ysb_, AF.Square, (Bysb_,), (B_ysqt,))
                    pM, BM = nbank()
                    pQ, BQ = nbank()
                    mmgroup([(pM[:, :], inv256[:], ysb_[:, ec, :], ec == 0, ec == 1) for ec in range(2)]
                            + [(pQ[:, :], inv256[:], ysqt[:, ec, :], ec == 0, ec == 1) for ec in range(2)], (Bysb_, B_ysqt, B_ones), (BM, BQ))
                    act_copy(mean_sb[:], pM[:, :], (BM,), (B_mean,))
                    rel(BM)
                    tt("dve", tmpE[:], mean_sb[:], mean_sb[:], ALU.mult, (B_mean,), (B_tE,))
                    tt("dve", tmpE[:], pQ[:, :], tmpE[:], ALU.subtract, (BQ, B_tE), (B_tE,))
                    rel(BQ)
                    ts2("dve", tmpE[:], tmpE[:], 1.0, EPS, ALU.mult, ALU.add, (B_tE,), (B_tE,))
                    act(tmpE[:], tmpE[:], AF.Sqrt, (B_tE,), (B_tE,))
                    recip(tmpE[:], tmpE[:], (B_tE,), (B_tE,))
                    gn = gvec(l, 4)
                    for ec in range(2):
                        e = 2 * h + ec
                        tt("dve", tmpF[:], ysb_[:, ec, :], mean_sb[:], ALU.subtract, (Bysb_, B_mean), (B_tF,))
                        tt("dve", tmpF[:], tmpF[:], tmpE[:], ALU.mult, (B_tF, B_tE), (B_tF,))
                        stt("dve", yrT[:, e, :], tmpF[:], gn[:, e:e + 1], silu[:, e, :], ALU.mult, ALU.mult, (B_tF, B_silu, B_pvec), (B_yr,))

                fillers += [(lambda h=h: ret_setup(h)) for h in range(R_HEADS)]
                for n_ in range(NB):
                    fillers.append(lambda n_=n_: [ret_chunk(n_, h_) for h_ in range(R_HEADS)])
                fillers += [(lambda h=h: ret_norm(h)) for h in range(R_HEADS)]
                nfill = len(fillers)
                sc_next = att_scores(0)
                for i in range(len(pairs)):
                    sc_cur = sc_next
                    if i + 1 < len(pairs):
                        sc_next = att_scores(i + 1)
                    for _ in range(3 if i < 2 else 2):
                        if fillers:
                            fillers.pop(0)()
                    att_rest(i, sc_cur)
                while fillers:
                    fillers.pop(0)()
                if t + 1 < n_tiles:
                    cpy("pool", kaT[:, l, 0:128], kaT[:, l, 512:640], (B_ka[l],), (B_ka[l],))
                    cpy("pool", vaT[:, l * 5, :], vaT[:, l * 5 + 4, :], (B_va[l],), (B_va[l],))

                if STOP_AFTER <= 4:
                    raise _Stop()
                for m in range(8):
                    w, Bw = next_w()
                    wr = w[:, 0:1024].rearrange("p (k n) -> p k n", k=8)
                    wga = w[:, 1024:2048].rearrange("p (k n) -> p k n", k=8)
                    wgg = w[:, 2048:3072].rearrange("p (k n) -> p k n", k=8)
                    wa = w[:, 3072:3584].rearrange("p (k n) -> p k n", k=4)
                    pGa, BGa = nbank()
                    pGg, BGg = nbank()
                    pA_, BA_ = nbank()
                    pR, BR = nbank()
                    mmgroup([(pGa[:, :], wga[:, k, :], hT[:, k, :], k == 0, k == 7) for k in range(8)], (Bw, B_h), (BGa,))
                    mmgroup([(pGg[:, :], wgg[:, k, :], hT[:, k, :], k == 0, k == 7) for k in range(8)], (Bw, B_h), (BGg,))
                    mmgroup([(pA_[:, :], wa[:, k, :], yaT[:, k, :], k == 0, k == 3) for k in range(4)], (Bw, B_ya), (BA_,))
                    mmgroup([(pR[:, :], wr[:, k, :], yrT[:, k, :], k == 0, k == 7) for k in range(8)], (Bw, B_yr), (BR,))
                    act(gtmp[0][:], pGa[:, :], AF.Sigmoid, (BGa,), (B_gt[0],))
                    act(gtmp[1][:], pGg[:, :], AF.Sigmoid, (BGg,), (B_gt[1],))
                    rel(BGa, BGg)
                    tt("dve", tmpA[:], pA_[:, :], gtmp[0][:], ALU.mult, (BA_, B_gt[0]), (B_tA,))
                    tt("dve", tmpB[:], pR[:, :], gtmp[1][:], ALU.mult, (BR, B_gt[1]), (B_tB,))
                    rel(BA_, BR)
                    tt("pool", merged[:, m, :], tmpA[:], tmpB[:], ALU.add, (B_tA, B_tB), (B_mg, B_vr))
                for mb in range(2):
                    w, Bw = next_w()
                    wv = w[:, 0:4096].rearrange("p (k n) -> p k n", k=8)
                    for jj in range(4):
                        m = mb * 4 + jj
                        pO, BO = nbank()
                        mmgroup([(pO[:, :], wv[:, k, jj * 128:(jj + 1) * 128], merged[:, k, :], k == 0, k == 7) for k in range(8)], (Bw, B_mg), (BO,))
                        act_copy(o_sb[:, m, :], pO[:, :], (BO,), (B_o[m], G_ffn))
                        act(sq[:, m, :], pO[:, :], AF.Square, (BO,), (B_sq[m],))
                        rel(BO)
                postnorm_residual(l, 1)
                if STOP_AFTER <= 5:
                    raise _Stop()

                prenorm(l, 2)
                cw = pvec[:, l * 128 + 40: l * 128 + 128].rearrange("p (f k) -> p f k", k=4)
                for j in range(11):
                    w, Bw = next_w()
                    wv = w[:, 0:4096].rearrange("p (k n) -> p k n", k=8)
                    pa = [proj_fm(wv, Bw, 0), proj_fm(wv, Bw, 1)]
                    pv = [proj_fm(wv, Bw, 2), proj_fm(wv, Bw, 3)]
                    for i in range(2):
                        f = 2 * j + i
                        pa_, Bpa = pa[i]
                        pv_, Bpv = pv[i]
                        ab, Bab = a_sb[i], B_asb[i]
                        ac, Bac = acc[i], B_acc[i]
                        act_copy(ab[:, 2:T + 2], pa_[:, :], (Bpa,), (Bab, G_o))
                        rel(Bpa)
                        cpy("pool", ab[:, 0:2], ccar[:, l * NF + f, :], (B_cc[l],), (Bab,))
                        if t + 1 < n_tiles:
                            cpy("pool", ccar[:, l * NF + f, :], ab[:, T:T + 2], (Bab,), (B_cc[l],))
                        ts2("dve", ac, ab[:, 2:T + 2], cw[:, f, 2:3], cw[:, f, 3:4], ALU.mult, ALU.add, (Bab, B_pvec), (Bac, G_o))
                        stt("dve", ac, ab[:, 1:T + 1], cw[:, f, 1:2], ac, ALU.mult, ALU.add, (Bab, Bac, B_pvec), (Bac,))
                        stt("dve", ac, ab[:, 0:T], cw[:, f, 0:1], ac, ALU.mult, ALU.add, (Bab, Bac, B_pvec), (Bac,))
                        act(gl[i], ac, AF.Gelu_apprx_tanh, (Bac,), (B_gl[i], G_o))
                        tt("dve", gated[:, f, :], gl[i], pv_[:, :], ALU.mult, (B_gl[i], Bpv), G_ar1)
                        rel(Bpv)
                for m in range(8):
                    w, Bw = next_w()
                    wv = w[:, 0:NF * 128].rearrange("p (k n) -> p k n", k=NF)
                    pO, BO = nbank()
                    mmgroup([(pO[:, :], wv[:, k, :], gated[:, k, :], k == 0, k == NF - 1) for k in range(NF)], (Bw, B_gated), (BO,))
                    act_copy(o_sb[:, m, :], pO[:, :], (BO,), (B_o[m], G_ffn))
                    act(sq[:, m, :], pO[:, :], AF.Square, (BO,), (B_sq[m],))
                    rel(BO)
                postnorm_residual(l, 3)
              except _Stop:
                pstate["live"].clear()
            dma("sp", yT_d[:, tsl].rearrange("(c p) t -> p c t", p=128), xT[:], "yo", B_x, ())
        P.ops["sp"].append(([("yo", P.cnt["yo"])], None, None, None))
        P.emit(nc, es)
    return nc


def _prep_shared(inputs, depth, n_tok):
    c = _constants(n_tok)
    f = lambda a: np.ascontiguousarray(np.asarray(a, dtype=np.float32))
    wall = np.stack([_pack_layer(f(inputs["w_in"][l]), f(inputs["w_out_a"][l]), f(inputs["w_out_r"][l]),
                                 f(inputs["w_out"][l]), f(inputs["w_up"][l]), f(inputs["w_down"][l])) for l in range(depth)])
    pv = np.zeros((128, depth * 128), np.float32)
    for l in range(depth):
        def pc(v):
            return f(v).reshape(8, 128).T
        blk = pv[:, l * 128:(l + 1) * 128]
        blk[:, 0:8] = pc(inputs["norm_pre_mix"][l])
        blk[:, 8:16] = pc(inputs["norm_post_mix"][l])
        blk[:, 16:24] = pc(inputs["norm_pre_ffn"][l])
        blk[:, 24:32] = pc(inputs["norm_post_ffn"][l])
        blk[:, 32:40] = pc(inputs["ret_norm"][l])
        cwv = f(inputs["conv_w"][l])
        cbv = f(inputs["conv_b"][l])
        cw4 = np.concatenate([cwv, cbv[None, :]], axis=0)
        blk[:, 40:128] = cw4.reshape(4, NF, 128).transpose(2, 1, 0).reshape(128, NF * 4)
    sinkb = np.ascontiguousarray(np.broadcast_to(f(inputs["sinks"])[:depth].reshape(1, depth * 8), (128, depth * 8)))
    shared = dict(wall=wall, pvec=pv, sinkb=sinkb, relb=f(inputs["rel_bias"]), oh=c["oh"], decay=c["decay"],
                  xib=c["xib"], zeta=c["zeta"], ident=c["ident"], cosT=c["cosT"], sinT=c["sinT"])
    return shared


def kernel(**inputs):
    x = np.asarray(inputs["x"], dtype=np.float32)
    B = x.shape[0]
    shared = _prep_shared(inputs, DEPTH, SEQ)
    nc = build(SEQ // T, DEPTH)
    in_maps = []
    for b in range(B):
        m = dict(shared)
        m["xT"] = np.ascontiguousarray(x[b].T)
        in_maps.append(m)
    res = run_bass_kernel_spmd(nc, in_maps, core_ids=list(range(B)))
    out = np.stack([np.ascontiguousarray(np.asarray(r["yT"], dtype=np.float32).T) for r in res.results], axis=0)
    return out.astype(np.float32)
```

```python
import math
from contextlib import ExitStack
import numpy as np
import concourse.bass as bass
import concourse.mybir as mybir
from concourse.bass_utils import run_bass_kernel_spmd

F32, BF16 = mybir.dt.float32, mybir.dt.bfloat16
ALU, AF = mybir.AluOpType, mybir.ActivationFunctionType

D = 1024
SEQ = 4096
DEPTH = 4
T = 512
NB = 4
A_HEADS, A_KV, A_HD = 8, 2, 64
R_HEADS, R_DK, R_DV = 4, 128, 256
D_FF = 2816
NF = 22
NUM_BUCKETS, MAX_DISTANCE = 32, 128
EPS = 1e-6
NEG = -30000.0
SEM_LIMIT = 30000
NWS = 4
DEBUG_TAGS = None
STOP_AFTER = 99
SKIP_SAME_ENGINE = False


class _Stop(Exception):
    pass

WSLOT = 4096

W1_GROUPS = [
    [("qa", 0), ("qa", 1), ("qa", 2), ("qa", 3)],
    [("ka", 0), ("va", 0), ("vr", 0), ("vr", 1)],
    [("vr", 2), ("vr", 3), ("vr", 4), ("vr", 5)],
    [("vr", 6), ("vr", 7), ("gr", 0), ("gr", 1)],
    [("qr", 0), ("qrs", 0), ("kr", 0), ("krs", 0)],
    [("qr", 1), ("qrs", 1), ("kr", 1), ("krs", 1)],
    [("qr", 2), ("qrs", 2), ("kr", 2), ("krs", 2)],
    [("qr", 3), ("qrs", 3), ("kr", 3), ("krs", 3)],
    [("gr", 2), ("gr", 3), ("gr", 4), ("gr", 5)],
    [("gr", 6), ("gr", 7)],
]


def _layer_groups():
    gs = []
    for roles in W1_GROUPS:
        gs.append(("w1", roles, 8 * 128 * len(roles)))
    for m in range(8):
        gs.append(("m", m, 8 * 128 * 3 + 4 * 128))
    for mb in range(2):
        gs.append(("wo", mb, 4096))
    for j in range(11):
        gs.append(("up", j, 4096))
    for m in range(8):
        gs.append(("dn", m, NF * 128))
    return gs


LAYER_GROUPS = _layer_groups()
WTOT = sum(g[2] for g in LAYER_GROUPS)


def _w1_cols(role, idx):
    A_Q, A_K = 512, 128
    o_qa, o_ka, o_va = 0, 512, 640
    o_qr, o_kr, o_vr, o_gr = 768, 1280, 1792, 2816
    ar = np.arange
    if role == "qa":
        return np.concatenate([o_qa + idx * 64 + ar(64), o_qa + (4 + idx) * 64 + ar(64)])
    if role == "ka":
        return o_ka + ar(128)
    if role == "va":
        return o_va + ar(128)
    if role == "qr":
        return o_qr + idx * 128 + ar(128)
    if role == "qrs":
        return o_qr + idx * 128 + np.concatenate([64 + ar(64), ar(64)])
    if role == "kr":
        return o_kr + idx * 128 + ar(128)
    if role == "krs":
        return o_kr + idx * 128 + np.concatenate([64 + ar(64), ar(64)])
    if role == "vr":
        return o_vr + idx * 128 + ar(128)
    if role == "gr":
        return o_gr + idx * 128 + ar(128)
    raise ValueError(role)


def _kchunks(w):
    K, n = w.shape
    return np.ascontiguousarray(w.reshape(K // 128, 128, n).transpose(1, 0, 2)).reshape(128, -1)


def _pack_layer(w_in, w_out_a, w_out_r, w_out, w_up, w_down):
    o_ga, o_gg = 3840, 4864
    parts = []
    rows_a = np.concatenate([np.concatenate([c * 64 + np.arange(64), (4 + c) * 64 + np.arange(64)]) for c in range(4)])
    w_oa = w_out_a[rows_a]
    for kind, arg, size in LAYER_GROUPS:
        if kind == "w1":
            cols = np.concatenate([_w1_cols(r, i) for r, i in arg])
            parts.append(_kchunks(w_in[:, cols]))
        elif kind == "m":
            m = arg
            cs = slice(m * 128, (m + 1) * 128)
            parts.append(np.concatenate([
                _kchunks(w_out_r[:, cs]),
                _kchunks(w_in[:, o_ga + m * 128:o_ga + (m + 1) * 128]),
                _kchunks(w_in[:, o_gg + m * 128:o_gg + (m + 1) * 128]),
                _kchunks(w_oa[:, cs]),
            ], axis=1))
        elif kind == "wo":
            parts.append(_kchunks(w_out[:, arg * 512:(arg + 1) * 512]))
        elif kind == "up":
            j = arg
            cols = np.concatenate([np.arange(2 * j * 128, (2 * j + 2) * 128), D_FF + np.arange(2 * j * 128, (2 * j + 2) * 128)])
            parts.append(_kchunks(w_up[:, cols]))
        elif kind == "dn":
            parts.append(_kchunks(w_down[:, arg * 128:(arg + 1) * 128]))
    out = np.concatenate(parts, axis=1)
    assert out.shape == (128, WTOT), out.shape
    return out


def _t5_bucket(n):
    max_exact = NUM_BUCKETS // 2
    n = np.maximum(n, 0)
    large = max_exact + (np.log(np.maximum(n, 1) / max_exact) / math.log(MAX_DISTANCE / max_exact)
                         * (NUM_BUCKETS - max_exact)).astype(np.int32)
    large = np.minimum(large, NUM_BUCKETS - 1)
    return np.where(n < max_exact, n, large).astype(np.int32)


def _constants(n_tok):
    c = {}
    oh = np.zeros((33, 2 * 255), np.float32)
    for pc in range(2):
        for i in range(255):
            dist = (127 - i) if pc == 1 else (255 - i)
            valid = (dist >= 0) and (dist < 128)
            b = int(_t5_bucket(np.array([dist]))[0]) if valid else 32
            oh[b, pc * 255 + i] = 1.0
    c["oh"] = oh
    lg = np.log(1.0 - 2.0 ** (-5.0 - np.arange(R_HEADS, dtype=np.float64)))
    idx = np.arange(128, dtype=np.float64)
    sc = R_DK ** -0.5
    rel = idx[None, :] - idx[:, None]
    dec = np.where(rel >= 0, np.exp(lg[:, None, None] * np.maximum(rel, 0.0)), 0.0) * sc
    c["decay"] = np.ascontiguousarray(dec.transpose(1, 0, 2)).reshape(128, 512).astype(np.float32)
    xi = np.exp(lg[:, None] * (idx + 1.0)[None, :])
    c["xib"] = np.ascontiguousarray(np.broadcast_to(xi.reshape(1, 512), (128, 512))).astype(np.float32)
    zeta = np.exp(lg[:, None] * (127 - idx)[None, :]) * sc
    c["zeta"] = np.ascontiguousarray(zeta.T).astype(np.float32)
    c["ident"] = np.eye(128, dtype=np.float32)
    half = 64
    freqs = (10000.0 ** (-np.arange(half, dtype=np.float32) / half)).astype(np.float32)
    ang = (np.arange(n_tok, dtype=np.float32)[None, :] * freqs[:, None]).astype(np.float32)
    cs, sn = np.cos(ang.astype(np.float64)), np.sin(ang.astype(np.float64))
    c["cosT"] = np.concatenate([cs, cs], axis=0).astype(np.float32)
    c["sinT"] = np.concatenate([-sn, sn], axis=0).astype(np.float32)
    return c


GAMMA_C = [float((1.0 - 2.0 ** (-5.0 - h)) ** 128) for h in range(R_HEADS)]


class Buf:
    __slots__ = ("name", "w", "r")

    def __init__(self, name):
        self.name = name
        self.w = None
        self.r = {}


class Prog:
    ENG = ("pe", "act", "dve", "pool", "sp")
    BLK = {"pe": "tensor", "act": "scalar", "dve": "vector", "pool": "gpsimd", "sp": "sync"}

    def __init__(self):
        self.ops = {e: [] for e in self.ENG}
        self.cnt = {}
        self.seen = {e: {} for e in self.ENG}
        self.epoch = {e: 0 for e in self.ENG}
        self.debug_tags = None

    def _need(self, e, ts, waits):
        if ts is None:
            return
        k, v = ts
        if self.seen[e].get(k, 0) >= v:
            return
        assert self.cnt.get(k, 0) >= v, ("unresolved timestamp", k, v)
        if e == "pe" and isinstance(k, tuple) and k[0] == "pe":
            return
        if SKIP_SAME_ENGINE and isinstance(k, tuple) and k[0] == e:
            return
        waits[k] = max(waits.get(k, 0), v)

    @staticmethod
    def _flat(x):
        out = []
        for b in x:
            if isinstance(b, Buf):
                out.append(b)
            else:
                out.extend(Prog._flat(b))
        return out

    def _waits(self, e, reads, writes):
        waits = {}
        for b in reads:
            self._need(e, b.w, waits)
        for b in writes:
            self._need(e, b.w, waits)
            for k, v in b.r.items():
                self._need(e, (k, v), waits)
        for k, v in waits.items():
            self.seen[e][k] = v
        return list(waits.items())

    def _mark(self, ts, reads, writes):
        k, v = ts
        for b in reads:
            if b.r.get(k, 0) < v:
                b.r[k] = v
        for b in writes:
            b.w = ts
            b.r = {}

    def op(self, e, fn, reads=(), writes=()):
        reads, writes = self._flat(reads), self._flat(writes)
        waits = self._waits(e, reads, writes)
        k = (e, self.epoch[e])
        self.cnt[k] = self.cnt.get(k, 0) + 1
        ts = (k, self.cnt[k])
        if self.cnt[k] >= SEM_LIMIT:
            self.epoch[e] += 1
        import sys as _s
        fr = _s._getframe(1)
        tags = []
        while fr is not None and len(tags) < 3:
            tags.append(fr.f_lineno)
            fr = fr.f_back
        self.ops[e].append((waits, fn, (k, 1), tags))
        self._mark(ts, reads, writes)

    def dma(self, q, fn, semkey, reads=(), writes=()):
        reads, writes = self._flat(reads), self._flat(writes)
        waits = self._waits(q, reads, writes)
        self.cnt[semkey] = self.cnt.get(semkey, 0) + 16
        ts = (semkey, self.cnt[semkey])
        self.ops[q].append((waits, fn, (semkey, 16), None))
        self._mark(ts, reads, writes)

    def wait_all(self, e, bufs):
        waits = self._waits(e, bufs, ())
        self.ops[e].append((waits, None, None, None))

    def emit(self, nc, es):
        keys = list(self.cnt.keys())
        sems = {}
        for i, k in enumerate(keys):
            sems[k] = es.enter_context(nc.semaphore("s%d" % i))
        block = es.enter_context(nc.Block())
        for e in self.ENG:
            oplist = self.ops[e]

            def body(eng, oplist=oplist):
                for waits, fn, inc, tags in oplist:
                    for k, v in waits:
                        eng.wait_ge(sems[k], v)
                    if fn is None:
                        continue
                    ins = fn(eng)
                    ins.then_inc(sems[inc[0]], inc[1])
                    if self.debug_tags is not None:
                        try:
                            self.debug_tags[str(ins.ins.name)] = tags
                        except Exception:
                            pass

            getattr(block, self.BLK[e])(body)


def build(n_tiles=8, depth=DEPTH):
    n_tok = n_tiles * T
    nc = bass.Bass("TRN2", target_bir_lowering=False)
    dr = {}

    def din(name, shape):
        dr[name] = nc.dram_tensor(name, list(shape), F32, kind="ExternalInput").ap()
        return dr[name]

    xT_d = din("xT", [D, n_tok])
    wall_d = din("wall", [depth, 128, WTOT])
    pvec_d = din("pvec", [128, depth * 128])
    sinkb_d = din("sinkb", [128, depth * 8])
    relb_d = din("relb", [32, 8])
    oh_d = din("oh", [33, 510])
    decay_d = din("decay", [128, 512])
    xib_d = din("xib", [128, 512])
    zeta_d = din("zeta", [128, 4])
    ident_d = din("ident", [128, 128])
    cos_d = din("cosT", [128, n_tok])
    sin_d = din("sinT", [128, n_tok])
    yT_d = nc.dram_tensor("yT", [D, n_tok], F32, kind="ExternalOutput").ap()

    P = Prog()
    if DEBUG_TAGS is not None:
        P.debug_tags = DEBUG_TAGS
    es = ExitStack()
    with es:
        def sb(name, shape, dt):
            return es.enter_context(nc.sbuf_tensor("s_" + name, list(shape), dt))

        def ps(name, shape, dt):
            return es.enter_context(nc.psum_tensor("p_" + name, list(shape), dt))

        xT = sb("xT", [128, 8, T], F32); B_x = [Buf("xT%d" % c) for c in range(8)]
        S32 = sb("S32", [128, depth * 4, 256], F32); B_S32 = [[Buf("S32") for _ in range(4)] for _ in range(depth)]
        Sbf = sb("Sbf", [128, 4, 256], BF16); B_Sbf = [Buf("Sbf") for _ in range(4)]
        kaT = sb("kaT", [128, depth, 640], BF16); B_ka = [Buf("ka") for _ in range(depth)]
        vaT = sb("vaT", [128, depth * 5, 128], BF16); B_va = [Buf("va") for _ in range(depth)]
        ccar = sb("ccar", [128, depth * NF, 2], F32); B_cc = [Buf("cc") for _ in range(depth)]
        bias = sb("bias", [128, 2 * 8, 128], F32); B_bias = Buf("bias")
        decay = sb("decay", [128, 512], F32); B_decay = Buf("decay")
        xib = sb("xib", [128, 512], F32); B_xib = Buf("xib")
        zeta = sb("zeta", [128, 4], F32); B_zeta = Buf("zeta")
        pvec = sb("pvec", [128, depth * 128], F32); B_pvec = Buf("pvec")
        esink = sb("esink", [128, depth * 8], F32); B_es = Buf("esink")
        identb = sb("identb", [128, 128], BF16); B_ident = Buf("ident")
        onesb = sb("onesb", [128, 128], BF16); B_ones = Buf("ones")
        inv256 = sb("inv256", [128, 128], BF16)
        onesQ = sb("onesQ", [128, 4, 128], BF16)
        selQ = sb("selQ", [128, 4, 128], BF16)
        esq = sb("esq", [128, depth * 2], F32); B_esq = Buf("esq")
        pk = [sb("pk%d" % i, [128, 128], F32) for i in range(2)]
        pkh = [sb("pkh%d" % i, [128, 128], BF16) for i in range(2)]
        pkh32 = [sb("pkh32_%d" % i, [128, 128], F32) for i in range(2)]
        pkl = [sb("pkl%d" % i, [128, 128], BF16) for i in range(2)]
        B_pk = [Buf("pk%d" % i) for i in range(2)]
        rb_sb = sb("rb_sb", [128, T], F32); B_rbs = Buf("rb_sb")
        dmy = sb("dmy", [128, 8], F32); B_dmy = Buf("dmy")
        cosS = sb("cosS", [128, T], F32); B_cos = Buf("cos")
        sinS = sb("sinS", [128, T], F32); B_sin = Buf("sin")
        wsl = [sb("ws%d" % i, [128, WSLOT], BF16) for i in range(NWS)]
        B_ws = [Buf("ws%d" % i) for i in range(NWS)]
        hT = sb("hT", [128, 8, T], BF16); B_h = Buf("hT")
        ar3 = sb("ar3", [128, 8 * T], BF16)
        sq = ar3[:, :].rearrange("p (c t) -> p c t", c=8)
        qaT = ar3[:, 0:4 * T].rearrange("p (c t) -> p c t", c=4)
        qrT = ar3[:, 4 * T:8 * T].rearrange("p (c t) -> p c t", c=4)
        B_sq = [Buf("sq%d" % c) for c in range(8)]
        B_qa = B_sq[0:4]
        B_qr = B_sq[4:8]
        ar1 = sb("ar1", [128, NF * T], BF16)
        gated = ar1[:, :].rearrange("p (f t) -> p f t", f=NF)
        q2T = ar1[:, 0:2048].rearrange("p (c t) -> p c t", c=4)
        krT = ar1[:, 2048:4096].rearrange("p (c t) -> p c t", c=4)
        PTa = [ar1[:, 4096 + i * 1024: 4096 + (i + 1) * 1024].rearrange("p (c t) -> p c t", c=2) for i in range(2)]
        PTr = ar1[:, 6144:6656]
        Kz = ar1[:, 6656:7168].rearrange("p (n d) -> p n d", n=4)
        yaT = ar1[:, 7168:9216].rearrange("p (c t) -> p c t", c=4)
        ysb = ar1[:, 9216:10240].rearrange("p (c t) -> p c t", c=2)
        ysq = ar1[:, 10240:11264].rearrange("p (c t) -> p c t", c=2)
        B_gated = Buf("gated"); B_q2 = Buf("q2"); B_kr = Buf("kr"); B_PTa = [Buf("PTa0"), Buf("PTa1")]
        B_PTr = Buf("PTr"); B_Kz = Buf("Kz"); B_ya = Buf("ya"); B_ysb = Buf("ysb"); B_ysq = Buf("ysq")
        G_ar1 = (B_gated, B_q2, B_kr, B_PTa[0], B_PTa[1], B_PTr, B_Kz, B_ya, B_ysb, B_ysq)
        tmpA = sb("tmpA", [128, T], F32); B_tA = Buf("tmpA")
        tmpB = sb("tmpB", [128, T], F32); B_tB = Buf("tmpB")
        tmpC = sb("tmpC", [128, T], BF16); B_tC = Buf("tmpC")
        tmpD = sb("tmpD", [128, T], BF16); B_tD = Buf("tmpD")
        tmpF = sb("tmpF", [128, T], F32); B_tF = Buf("tmpF")
        PTrX = sb("PTrX", [128, 3, T], BF16)
        KzX = sb("KzX", [128, 3 * 4, 128], BF16)
        ysb2 = sb("ysb2", [128, 2, T], BF16)
        ysq2 = sb("ysq2", [128, 2, T], BF16)
        ysqt = sb("ysqt", [128, 2, T], BF16); B_ysqt = Buf("ysqt")
        arV = sb("arV", [128, 4096], BF16)
        vr = arV[:, :].rearrange("p (n e) -> p n e", n=4)
        merged = arV[:, :].rearrange("p (c t) -> p c t", c=8)
        B_vr = Buf("vr"); B_mg = Buf("merged")
        silu = sb("silu", [128, 8, T], BF16); B_silu = Buf("silu")
        yrT = sb("yrT", [128, 8, T], BF16); B_yr = Buf("yr")
        gtmp = [sb("gtmp%d" % i, [128, T], BF16) for i in range(2)]; B_gt = [Buf("gt%d" % i) for i in range(2)]
        arF = sb("arF", [128, 8 * T], F32)
        o_sb = arF[:, :].rearrange("p (c t) -> p c t", c=8)
        a_sb = [arF[:, i * 516: i * 516 + T + 2] for i in range(2)]
        acc = [arF[:, 1032 + i * T: 1032 + (i + 1) * T] for i in range(2)]
        gl = [arF[:, 2056 + i * T: 2056 + (i + 1) * T] for i in range(2)]
        B_o = [Buf("o%d" % c) for c in range(8)]
        B_asb = [Buf("asb%d" % i) for i in range(2)]; B_acc = [Buf("acc%d" % i) for i in range(2)]; B_gl = [Buf("gl%d" % i) for i in range(2)]
        G_ffn = tuple(B_asb + B_acc + B_gl)
        tmpE4 = arF[:, 0:4 * T].rearrange("p (c t) -> p c t", c=4); B_tE4 = B_o[0:4]
        mean4 = arF[:, 4 * T:8 * T].rearrange("p (c t) -> p c t", c=4); B_mean4 = B_o[4:8]
        G_o = tuple(B_o)
        PTrs = [PTr] + [PTrX[:, i, :] for i in range(3)]; B_PTrs = [(B_PTr, B_gated)] + [(Buf("PTrX%d" % i),) for i in range(3)]
        Kzs = [Kz] + [KzX[:, 4 * i:4 * i + 4, :] for i in range(3)]; B_Kzs = [(B_Kz, B_gated)] + [(Buf("KzX%d" % i),) for i in range(3)]
        ysbs = [ysb, ysq, ysb2[:, :, :], ysq2[:, :, :]]; B_ysbs = [(B_ysb, B_gated), (B_ysq, B_gated), (Buf("ysb2"),), (Buf("ysq2"),)]
        tab = sb("tab", [33, 8], F32)
        tabh = sb("tabh", [33, 8], BF16)
        tabh32 = sb("tabh32", [33, 8], F32)
        tabl = sb("tabl", [33, 8], BF16)
        ohb = sb("ohb", [33, 510], BF16); B_oh = Buf("oh")
        B_tab = Buf("tab")
        NPB = 7
        pbank = [ps("pb%d" % i, [128, 512], F32) for i in range(NPB)]
        B_pb = [Buf("pb%d" % i) for i in range(NPB)]
        ptr = ps("ptr", [128, 1024], BF16); B_ptr = Buf("ptr")
        pstate = {"i": 0, "live": set()}

        def nbank():
            for d_ in range(NPB):
                i = (pstate["i"] + d_) % NPB
                if i not in pstate["live"]:
                    pstate["i"] = (i + 1) % NPB
                    pstate["live"].add(i)
                    return pbank[i], B_pb[i]
            raise RuntimeError("no free PSUM bank")

        def rel(*bs):
            for b_ in bs:
                pstate["live"].discard(B_pb.index(b_))

        def mmgroup(specs, reads, writes):
            def fn(eng, specs=specs):
                ins = None
                for (o, l_, r_, st, sp_) in specs:
                    ins = eng.matmul(o, lhsT=l_, rhs=r_, start=st, stop=sp_)
                return ins
            P.op("pe", fn, reads, writes)

        def transposes(specs, reads, writes):
            def fn(eng, specs=specs):
                ins = None
                for (o, i_) in specs:
                    ins = eng.transpose(out=o, in_=i_, identity=identb[:])
                return ins
            P.op("pe", fn, reads, writes)

        def act(out, in_, func, reads, writes, scale=None):
            def fn(eng):
                if scale is None:
                    return eng.activation(out=out, in_=in_, func=func)
                return eng.activation(out=out, in_=in_, func=func, scale=scale)
            P.op("act", fn, reads, writes)

        def act_mul(out, in_, mul, reads, writes):
            P.op("act", lambda eng: eng.mul(out=out, in_=in_, mul=mul), reads, writes)

        def act_copy(out, in_, reads, writes):
            P.op("act", lambda eng: eng.copy(out=out, in_=in_), reads, writes)

        def tt(e, out, in0, in1, op, reads, writes):
            P.op(e, lambda eng: eng.tensor_tensor(out=out, in0=in0, in1=in1, op=op), reads, writes)

        def ts2(e, out, in0, s1, s2, op0, op1, reads, writes):
            P.op(e, lambda eng: eng.tensor_scalar(out=out, in0=in0, scalar1=s1, scalar2=s2, op0=op0, op1=op1), reads, writes)

        def stt(e, out, in0, scalar, in1, op0, op1, reads, writes):
            P.op(e, lambda eng: eng.scalar_tensor_tensor(out=out, in0=in0, scalar=scalar, in1=in1, op0=op0, op1=op1), reads, writes)

        def cpy(e, out, in_, reads, writes):
            P.op(e, lambda eng: eng.tensor_copy(out=out, in_=in_), reads, writes)

        def recip(out, in_, reads, writes):
            P.op("dve", lambda eng: eng.reciprocal(out=out, in_=in_), reads, writes)

        def memset(e, ap, val, writes):
            P.op(e, lambda eng: eng.memset(ap, val), (), writes)

        def dma(q, out, in_, semkey, reads, writes):
            P.dma(q, lambda eng: eng.dma_start(out=out, in_=in_), semkey, reads, writes)

        gseq = []
        for t in range(n_tiles):
            for l in range(depth):
                off = 0
                for (kind, arg, size) in LAYER_GROUPS:
                    gseq.append((l, off, size))
                    off += size
        wstate = {"issued": 0, "used": 0}

        def issue_w():
            g = wstate["issued"]
            if g >= len(gseq):
                return
            l_, off, size = gseq[g]
            s_ = g % NWS
            dma("pool", wsl[s_][:, 0:size], wall_d[l_, :, off:off + size], "w%d" % s_, (), (B_ws[s_],))
            wstate["issued"] = g + 1

        def next_w():
            g = wstate["used"]
            wstate["used"] = g + 1
            while wstate["issued"] < min(g + NWS, len(gseq)):
                issue_w()
            s_ = g % NWS
            return wsl[s_], B_ws[s_]

        dma("sp", decay[:], decay_d, "su0", (), (B_decay,))
        dma("sp", xib[:], xib_d, "su1", (), (B_xib,))
        dma("sp", zeta[:], zeta_d, "su2", (), (B_zeta,))
        dma("sp", pvec[:], pvec_d, "su3", (), (B_pvec,))
        dma("sp", esink[:], sinkb_d, "su4", (), (B_es,))
        dma("sp", tab[0:32, :], relb_d, "su5", (), (B_tab,))
        dma("pool", identb[:], ident_d, "su6", (), (B_ident,))
        dma("pool", ohb[:], oh_d, "su7", (), (B_oh,))
        for _ in range(NWS - 1):
            issue_w()
        memset("dve", onesb[:], 1.0, (B_ones,))
        memset("dve", inv256[:], 1.0 / 256.0, (B_ones,))
        memset("dve", onesQ[:], 0.0, (B_ones,))
        memset("dve", selQ[:], 0.0, (B_ones,))
        for n_ in range(4):
            memset("dve", onesQ[:, n_, 32 * n_:32 * n_ + 32], 1.0, (B_ones,))
            memset("dve", selQ[32 * n_:32 * n_ + 1, n_, :], 1.0, (B_ones,))
        memset("dve", dmy[:], 1.0, (B_dmy,))
        memset("dve", S32[:], 0.0, B_S32)
        memset("dve", kaT[:], 0.0, B_ka)
        memset("dve", vaT[:], 0.0, B_va)
        memset("dve", ccar[:], 0.0, B_cc)
        memset("dve", tab[32:33, :], NEG, (B_tab,))
        act(esink[:], esink[:], AF.Exp, (B_es,), (B_es,))
        for l_ in range(depth):
            for j_ in range(2):
                for c_ in range(4):
                    col = l_ * 8 + j_ * 4 + c_
                    cpy("dve", esq[32 * c_:32 * c_ + 32, l_ * 2 + j_:l_ * 2 + j_ + 1], esink[32 * c_:32 * c_ + 32, col:col + 1], (B_es,), (B_esq,))
        cpy("dve", tabh[:], tab[:], (B_tab,), (B_tab,))
        cpy("dve", tabh32[:], tabh[:], (B_tab,), (B_tab,))
        tt("dve", tabh32[:], tab[:], tabh32[:], ALU.subtract, (B_tab,), (B_tab,))
        cpy("dve", tabl[:], tabh32[:], (B_tab,), (B_tab,))
        for b4 in range(4):
            pc, q0 = b4 // 2, (b4 % 2) * 64
            pb_, Bp = nbank()
            specs = []
            for qq in range(64):
                q = q0 + qq
                lh = ohb[:, pc * 255 + 127 - q: pc * 255 + 255 - q]
                specs.append((pb_[:, qq * 8:(qq + 1) * 8], lh, tabh[:], True, False))
                specs.append((pb_[:, qq * 8:(qq + 1) * 8], lh, tabl[:], False, True))
            mmgroup(specs, (B_tab, B_oh), (Bp,))
            cpy("dve", bias[:, pc * 8:(pc + 1) * 8, q0:q0 + 64], pb_[:, :].rearrange("p (q h) -> p h q", h=8), (Bp,), (B_bias,))
            rel(Bp)

        def gvec(l, j):
            return pvec[:, l * 128 + j * 8: l * 128 + (j + 1) * 8]

        pkstate = {"i": 0}

        def packed_recip_bcast(pkp, Bpkp, scale, addc, do_sqrt):
            i_ = pkstate["i"]
            pkstate["i"] = 1 - i_
            p_, ph_, ph32_, pl_, Bp_ = pk[i_], pkh[i_], pkh32[i_], pkl[i_], B_pk[i_]
            rd = (Bpkp, B_esq) if not isinstance(addc, float) else (Bpkp,)
            ts2("dve", p_[:], pkp, scale, addc, ALU.mult, ALU.add, rd, (Bp_,))
            rel(Bpkp)
            if do_sqrt:
                act(p_[:], p_[:], AF.Sqrt, (Bp_,), (Bp_,))
            recip(p_[:], p_[:], (Bp_,), (Bp_,))
            cpy("dve", ph_[:], p_[:], (Bp_,), (Bp_,))
            cpy("dve", ph32_[:], ph_[:], (Bp_,), (Bp_,))
            tt("dve", pl_[:], p_[:], ph32_[:], ALU.subtract, (Bp_,), (Bp_,))
            rb, Brb = nbank()
            specs = []
            for n in range(4):
                specs.append((rb[:, n * 128:(n + 1) * 128], selQ[:, n, :], ph_[:], True, False))
                specs.append((rb[:, n * 128:(n + 1) * 128], selQ[:, n, :], pl_[:], False, True))
            mmgroup(specs, (Bp_, B_ones), (Brb,))
            return rb, Brb

        def preload(func):
            act(dmy[:, 0:1], dmy[:, 1:2], func, (), (B_dmy,))

        def rms_rstd():
            preload(AF.Sqrt)
            pb_, Bp = nbank()
            specs = []
            for n in range(4):
                for c in range(8):
                    specs.append((pb_[:, 0:128], onesQ[:, n, :], sq[:, c, n * 128:(n + 1) * 128], n == 0 and c == 0, n == 3 and c == 7))
            mmgroup(specs, (B_sq, B_ones), (Bp,))
            return packed_recip_bcast(pb_[:, 0:128], Bp, 1.0 / D, EPS, True)

        def prenorm(l, j):
            for c in range(8):
                act(sq[:, c, :], xT[:, c, :], AF.Square, (B_x[c],), (B_sq[c],))
            rb, Brb = rms_rstd()
            g = gvec(l, j)
            for c in range(8):
                stt("dve", hT[:, c, :], xT[:, c, :], g[:, c:c + 1], rb[:, :], ALU.mult, ALU.mult, (B_x[c], Brb, B_pvec), (B_h,))
            rel(Brb)

        def postnorm_residual(l, j):
            rb, Brb = rms_rstd()
            g = gvec(l, j)
            for c in range(8):
                stt("dve", o_sb[:, c, :], o_sb[:, c, :], g[:, c:c + 1], rb[:, :], ALU.mult, ALU.mult, (B_o[c], Brb, B_pvec), (B_o[c],))
                tt("dve", xT[:, c, :], xT[:, c, :], o_sb[:, c, :], ALU.add, (B_x[c], B_o[c]), (B_x[c],))
            rel(Brb)

        def proj_fm(wv, Bw, j):
            pb_, Bp = nbank()
            specs = [(pb_[:, :], wv[:, k, j * 128:(j + 1) * 128], hT[:, k, :], k == 0, k == 7) for k in range(8)]
            mmgroup(specs, (Bw, B_h), (Bp,))
            return pb_, Bp

        def proj_tm(wv, Bw, j):
            pb_, Bp = nbank()
            specs = []
            for n in range(NB):
                for k in range(8):
                    specs.append((pb_[:, n * 128:(n + 1) * 128], hT[:, k, n * 128:(n + 1) * 128], wv[:, k, j * 128:(j + 1) * 128], k == 0, k == 7))
            mmgroup(specs, (Bw, B_h), (Bp,))
            return pb_, Bp

        def do_w1_group(l, roles):
            w, Bw = next_w()
            nch = len(roles)
            wv = w[:, 0:8 * 128 * nch].rearrange("p (k n) -> p k n", k=8)
            j = 0
            while j < nch:
                role, idx = roles[j]
                if role == "qa":
                    pb_, Bp = proj_fm(wv, Bw, j)
                    act_mul(qaT[:, idx, :], pb_[:, :], A_HD ** -0.5, (Bp,), (B_qa[idx],))
                    rel(Bp)
                elif role == "ka":
                    pb_, Bp = proj_fm(wv, Bw, j)
                    act_copy(kaT[:, l, 128:640], pb_[:, :], (Bp,), (B_ka[l],))
                    rel(Bp)
                elif role == "va":
                    pb_, Bp = proj_tm(wv, Bw, j)
                    act_copy(vaT[:, l * 5 + 1:l * 5 + 5, :], pb_[:, :].rearrange("p (n c) -> p n c", n=4), (Bp,), (B_va[l],))
                    rel(Bp)
                elif role == "vr":
                    pb_, Bp = proj_tm(wv, Bw, j)
                    act_copy(vr[:, :, idx * 128:(idx + 1) * 128], pb_[:, :].rearrange("p (n c) -> p n c", n=4), (Bp,), (B_vr, B_mg))
                    rel(Bp)
                elif role == "gr":
                    pb_, Bp = proj_fm(wv, Bw, j)
                    act(silu[:, idx, :], pb_[:, :], AF.Silu, (Bp,), (B_silu,))
                    rel(Bp)
                elif role in ("qr", "kr"):
                    pq, Bq = proj_fm(wv, Bw, j)
                    pq2, Bq2 = proj_fm(wv, Bw, j + 1)
                    tt("dve", tmpC[:], pq[:, :], cosS[:], ALU.mult, (Bq, B_cos), (B_tC,))
                    tt("dve", tmpD[:], pq2[:, :], sinS[:], ALU.mult, (Bq2, B_sin), (B_tD,))
                    rel(Bq, Bq2)
                    if role == "qr":
                        tt("pool", qrT[:, idx, :], tmpC[:], tmpD[:], ALU.add, (B_tC, B_tD), (B_qr[idx],))
                        tt("pool", q2T[:, idx, :].rearrange("p (n c) -> p n c", n=4), qrT[:, idx, :].rearrange("p (n c) -> p n c", n=4),
                           bass.AP(xib, idx * 128, [[512, 128], [0, 4], [1, 128]]), ALU.mult, (B_qr[idx], B_xib), (B_q2, B_gated))
                    else:
                        tt("pool", krT[:, idx, :], tmpC[:], tmpD[:], ALU.add, (B_tC, B_tD), (B_kr, B_gated))
                    j += 1
                else:
                    raise ValueError(role)
                j += 1

        for t in range(n_tiles):
            tsl = slice(t * T, (t + 1) * T)
            dma("sp", xT[:], xT_d[:, tsl].rearrange("(c p) t -> p c t", p=128), "xl", (), B_x)
            dma("sp", cosS[:], cos_d[:, tsl], "cl", (), (B_cos,))
            dma("sp", sinS[:], sin_d[:, tsl], "sl", (), (B_sin,))
            for l in range(depth):
              try:
                first_blk = (t == 0)
                if STOP_AFTER <= 0:
                    raise _Stop()
                prenorm(l, 0)
                if STOP_AFTER <= 1:
                    raise _Stop()
                for gi in range(4):
                    do_w1_group(l, W1_GROUPS[gi])
                fillers = [(lambda gi=gi, l=l: do_w1_group(l, W1_GROUPS[gi])) for gi in range(4, len(W1_GROUPS))]
                if STOP_AFTER <= 2:
                    raise _Stop()

                pairs = [(n, j) for n in range(NB) for j in range(A_KV)]

                def att_scores(i):
                    n, j = pairs[i]
                    prs = slice(j * 64, (j + 1) * 64)
                    out = []
                    for pc in range(2):
                        if pc == 0 and first_blk and n == 0:
                            out.append(None)
                            continue
                        pb_, Bp = nbank()
                        kc = slice((n + pc) * 128, (n + pc + 1) * 128)
                        specs = [(pb_[:, :], kaT[prs, l, kc], qaT[prs, :, n * 128:(n + 1) * 128], True, True)]
                        mmgroup(specs, (B_ka[l], B_qa), (Bp,))
                        out.append((pb_, Bp))
                    return out

                def att_rest(i, sc):
                    n, j = pairs[i]
                    prs = slice(j * 64, (j + 1) * 64)
                    pt, Bpt = PTa[i % 2], B_PTa[i % 2]
                    used = []
                    for pc in range(2):
                        if sc[pc] is None:
                            continue
                        pb_, Bp = sc[pc]
                        bview = bias[:, pc * 8 + j * 4: pc * 8 + j * 4 + 4, :]
                        tt("dve", tmpA[:].rearrange("p (c q) -> p c q", c=4), pb_[:, :].rearrange("p (c q) -> p c q", c=4), bview, ALU.add, (Bp, B_bias), (B_tA,))
                        rel(Bp)
                        act(pt[:, pc, :], tmpA[:], AF.Exp, (B_tA,), (Bpt, B_gated))
                        used.append(pc)
                    pn, Bn = nbank()
                    pd, Bd = nbank()
                    specs = []
                    for ii, pc in enumerate(used):
                        specs.append((pn[:, :], vaT[:, l * 5 + n + pc, :], pt[:, pc, :], ii == 0, ii == len(used) - 1))
                    nd_ = len(used) * 4
                    k_ = 0
                    for pc in used:
                        for c in range(4):
                            specs.append((pd[:, 0:128], onesQ[:, c, :], pt[:, pc, c * 128:(c + 1) * 128], k_ == 0, k_ == nd_ - 1))
                            k_ += 1
                    mmgroup(specs, (B_va[l], Bpt, B_ones), (Bn, Bd))
                    rb, Brb = packed_recip_bcast(pd[:, 0:128], Bd, 1.0, esq[:, l * 2 + j:l * 2 + j + 1], False)
                    act_copy(rb_sb[prs, :], rb[prs, :], (Brb,), (B_rbs,))
                    rel(Brb)
                    tt("dve", yaT[prs, :, n * 128:(n + 1) * 128], pn[prs, :].rearrange("p (c q) -> p c q", c=4),
                       rb_sb[prs, :].rearrange("p (c q) -> p c q", c=4), ALU.mult, (Bn, B_rbs), (B_ya, B_gated))
                    rel(Bn)

                def ret_setup(h):
                    PTr_, BPTr_, Kz_, BKz_ = PTrs[h], B_PTrs[h], Kzs[h], B_Kzs[h]
                    pA, BA = nbank()
                    specs = [(pA[:, n * 128:(n + 1) * 128], krT[:, h, n * 128:(n + 1) * 128], qrT[:, h, n * 128:(n + 1) * 128], True, True) for n in range(NB)]
                    mmgroup(specs, (B_kr, B_qr[h]), (BA,))
                    tt("dve", PTr_.rearrange("p (n c) -> p n c", n=4), pA[:, :].rearrange("p (n c) -> p n c", n=4),
                       bass.AP(decay, h * 128, [[512, 128], [0, 4], [1, 128]]), ALU.mult, (BA, B_decay), (BPTr_,))
                    rel(BA)
                    transposes([(ptr[:, n * 128:(n + 1) * 128], krT[:, h, n * 128:(n + 1) * 128]) for n in range(NB)], (B_kr, B_ident), (B_ptr,))
                    act_mul(Kz_.rearrange("p n d -> p (n d)"), ptr[:, 0:512], zeta[:, h:h + 1], (B_ptr, B_zeta), (BKz_,))

                def ret_row(n):
                    ret_half(n, (0, 1))
                    ret_half(n, (2, 3))

                def ret_half(n, heads):
                    banks = {}
                    for h in heads:
                        act_copy(Sbf[:, h, :], S32[:, l * 4 + h, :], (B_S32[l][h],), (B_Sbf[h],))
                    for h in heads:
                        PTr_, BPTr_, Kz_, BKz_ = PTrs[h], B_PTrs[h], Kzs[h], B_Kzs[h]
                        pY, BY = nbank()
                        specs = []
                        for ec in range(2):
                            o = pY[:, ec * 128:(ec + 1) * 128]
                            specs.append((o, vr[:, n, h * 256 + ec * 128: h * 256 + (ec + 1) * 128], PTr_[:, n * 128:(n + 1) * 128], True, False))
                            specs.append((o, Sbf[:, h, ec * 128:(ec + 1) * 128], q2T[:, h, n * 128:(n + 1) * 128], False, True))
                        specs.append((pY[:, 256:512], Kz_[:, n, :], vr[:, n, h * 256:(h + 1) * 256], True, True))
                        mmgroup(specs, (B_vr, BPTr_, B_Sbf[h], B_q2, BKz_), (BY,))
                        banks[h] = (pY, BY)
                    for h in heads:
                        pY, BY = banks[h]
                        stt("dve", S32[:, l * 4 + h, :], S32[:, l * 4 + h, :], GAMMA_C[h], pY[:, 256:512], ALU.mult, ALU.add, (B_S32[l][h], BY), (B_S32[l][h], BY))
                    for h in heads:
                        pY, BY = banks[h]
                        act_copy(ysbs[h][:, :, n * 128:(n + 1) * 128], pY[:, 0:256].rearrange("p (e c) -> p e c", e=2), (BY,), (B_ysbs[h],))
                        rel(BY)

                def ret_norm_stats(h):
                    ysb_, Bysb_ = ysbs[h], B_ysbs[h]
                    tE, BtE, mn, Bmn = tmpE4[:, h, :], B_tE4[h], mean4[:, h, :], B_mean4[h]
                    act(ysqt[:], ysb_, AF.Square, (Bysb_,), (B_ysqt,))
                    pM, BM = nbank()
                    pQ, BQ = nbank()
                    mmgroup([(pM[:, :], inv256[:], ysb_[:, ec, :], ec == 0, ec == 1) for ec in range(2)]
                            + [(pQ[:, :], inv256[:], ysqt[:, ec, :], ec == 0, ec == 1) for ec in range(2)], (Bysb_, B_ysqt, B_ones), (BM, BQ))
                    act_copy(mn, pM[:, :], (BM,), (Bmn,))
                    rel(BM)
                    tt("dve", tE, mn, mn, ALU.mult, (Bmn,), (BtE,))
                    tt("dve", tE, pQ[:, :], tE, ALU.subtract, (BQ, BtE), (BtE,))
                    rel(BQ)
                    ts2("dve", tE, tE, 1.0, EPS, ALU.mult, ALU.add, (BtE,), (BtE,))
                    act(tE, tE, AF.Sqrt, (BtE,), (BtE,))
                    recip(tE, tE, (BtE,), (BtE,))

                def ret_norm_apply(h):
                    ysb_, Bysb_ = ysbs[h], B_ysbs[h]
                    tE, BtE, mn, Bmn = tmpE4[:, h, :], B_tE4[h], mean4[:, h, :], B_mean4[h]
                    gn = gvec(l, 4)
                    for ec in range(2):
                        e = 2 * h + ec
                        tt("dve", tmpF[:], ysb_[:, ec, :], mn, ALU.subtract, (Bysb_, Bmn), (B_tF,))
                        tt("dve", tmpF[:], tmpF[:], tE, ALU.mult, (B_tF, BtE), (B_tF,))
                        stt("dve", yrT[:, e, :], tmpF[:], gn[:, e:e + 1], silu[:, e, :], ALU.mult, ALU.mult, (B_tF, B_silu, B_pvec), (B_yr,))

                fillers += [(lambda h=h: ret_setup(h)) for h in range(R_HEADS)]
                for n_ in range(NB):
                    fillers.append(lambda n_=n_: ret_row(n_))
                nfill = len(fillers)
                sc_next = att_scores(0)
                for i in range(len(pairs)):
                    sc_cur = sc_next
                    if i + 1 < len(pairs):
                        sc_next = att_scores(i + 1)
                    for _ in range(2):
                        if fillers:
                            fillers.pop(0)()
                    att_rest(i, sc_cur)
                while fillers:
                    fillers.pop(0)()
                preload(AF.Sqrt)
                for h in range(R_HEADS):
                    ret_norm_stats(h)
                for h in range(R_HEADS):
                    ret_norm_apply(h)
                if t + 1 < n_tiles:
                    cpy("pool", kaT[:, l, 0:128], kaT[:, l, 512:640], (B_ka[l],), (B_ka[l],))
                    cpy("pool", vaT[:, l * 5, :], vaT[:, l * 5 + 4, :], (B_va[l],), (B_va[l],))

                if STOP_AFTER <= 4:
                    raise _Stop()
                for m in range(8):
                    w, Bw = next_w()
                    wr = w[:, 0:1024].rearrange("p (k n) -> p k n", k=8)
                    wga = w[:, 1024:2048].rearrange("p (k n) -> p k n", k=8)
                    wgg = w[:, 2048:3072].rearrange("p (k n) -> p k n", k=8)
                    wa = w[:, 3072:3584].rearrange("p (k n) -> p k n", k=4)
                    pGa, BGa = nbank()
                    pGg, BGg = nbank()
                    pA_, BA_ = nbank()
                    pR, BR = nbank()
                    mmgroup([(pGa[:, :], wga[:, k, :], hT[:, k, :], k == 0, k == 7) for k in range(8)], (Bw, B_h), (BGa,))
                    mmgroup([(pGg[:, :], wgg[:, k, :], hT[:, k, :], k == 0, k == 7) for k in range(8)], (Bw, B_h), (BGg,))
                    mmgroup([(pA_[:, :], wa[:, k, :], yaT[:, k, :], k == 0, k == 3) for k in range(4)], (Bw, B_ya), (BA_,))
                    mmgroup([(pR[:, :], wr[:, k, :], yrT[:, k, :], k == 0, k == 7) for k in range(8)], (Bw, B_yr), (BR,))
                    act(gtmp[0][:], pGa[:, :], AF.Sigmoid, (BGa,), (B_gt[0],))
                    act(gtmp[1][:], pGg[:, :], AF.Sigmoid, (BGg,), (B_gt[1],))
                    rel(BGa, BGg)
                    tt("dve", tmpA[:], pA_[:, :], gtmp[0][:], ALU.mult, (BA_, B_gt[0]), (B_tA,))
                    tt("dve", tmpB[:], pR[:, :], gtmp[1][:], ALU.mult, (BR, B_gt[1]), (B_tB,))
                    rel(BA_, BR)
                    tt("pool", merged[:, m, :], tmpA[:], tmpB[:], ALU.add, (B_tA, B_tB), (B_mg, B_vr))
                for mb in range(2):
                    w, Bw = next_w()
                    wv = w[:, 0:4096].rearrange("p (k n) -> p k n", k=8)
                    for jj in range(4):
                        m = mb * 4 + jj
                        pO, BO = nbank()
                        mmgroup([(pO[:, :], wv[:, k, jj * 128:(jj + 1) * 128], merged[:, k, :], k == 0, k == 7) for k in range(8)], (Bw, B_mg), (BO,))
                        act_copy(o_sb[:, m, :], pO[:, :], (BO,), (B_o[m], G_ffn))
                        act(sq[:, m, :], pO[:, :], AF.Square, (BO,), (B_sq[m],))
                        rel(BO)
                postnorm_residual(l, 1)
                if STOP_AFTER <= 5:
                    raise _Stop()

                prenorm(l, 2)
                cw = pvec[:, l * 128 + 40: l * 128 + 128].rearrange("p (f k) -> p f k", k=4)
                for j in range(11):
                    w, Bw = next_w()
                    wv = w[:, 0:4096].rearrange("p (k n) -> p k n", k=8)
                    pa = [proj_fm(wv, Bw, 0), proj_fm(wv, Bw, 1)]
                    pv = [proj_fm(wv, Bw, 2), proj_fm(wv, Bw, 3)]
                    for i in range(2):
                        f = 2 * j + i
                        pa_, Bpa = pa[i]
                        pv_, Bpv = pv[i]
                        ab, Bab = a_sb[i], B_asb[i]
                        ac, Bac = acc[i], B_acc[i]
                        act_copy(ab[:, 2:T + 2], pa_[:, :], (Bpa,), (Bab, G_o))
                        rel(Bpa)
                        cpy("pool", ab[:, 0:2], ccar[:, l * NF + f, :], (B_cc[l],), (Bab,))
                        if t + 1 < n_tiles:
                            cpy("pool", ccar[:, l * NF + f, :], ab[:, T:T + 2], (Bab,), (B_cc[l],))
                        ts2("dve", ac, ab[:, 2:T + 2], cw[:, f, 2:3], cw[:, f, 3:4], ALU.mult, ALU.add, (Bab, B_pvec), (Bac, G_o))
                        stt("dve", ac, ab[:, 1:T + 1], cw[:, f, 1:2], ac, ALU.mult, ALU.add, (Bab, Bac, B_pvec), (Bac,))
                        stt("dve", ac, ab[:, 0:T], cw[:, f, 0:1], ac, ALU.mult, ALU.add, (Bab, Bac, B_pvec), (Bac,))
                        act(gl[i], ac, AF.Gelu_apprx_tanh, (Bac,), (B_gl[i], G_o))
                        tt("dve", gated[:, f, :], gl[i], pv_[:, :], ALU.mult, (B_gl[i], Bpv), G_ar1)
                        rel(Bpv)
                for m in range(8):
                    w, Bw = next_w()
                    wv = w[:, 0:NF * 128].rearrange("p (k n) -> p k n", k=NF)
                    pO, BO = nbank()
                    mmgroup([(pO[:, :], wv[:, k, :], gated[:, k, :], k == 0, k == NF - 1) for k in range(NF)], (Bw, B_gated), (BO,))
                    act_copy(o_sb[:, m, :], pO[:, :], (BO,), (B_o[m], G_ffn))
                    act(sq[:, m, :], pO[:, :], AF.Square, (BO,), (B_sq[m],))
                    rel(BO)
                postnorm_residual(l, 3)
              except _Stop:
                pstate["live"].clear()
            dma("sp", yT_d[:, tsl].rearrange("(c p) t -> p c t", p=128), xT[:], "yo", B_x, ())
        P.ops["sp"].append(([("yo", P.cnt["yo"])], None, None, None))
        if DEBUG_TAGS is not None:
            print("sbuf bytes remaining", nc.sbuf_bytes_remaining)
        P.emit(nc, es)
    return nc


def _prep_shared(inputs, depth, n_tok):
    c = _constants(n_tok)
    f = lambda a: np.ascontiguousarray(np.asarray(a, dtype=np.float32))
    wall = np.stack([_pack_layer(f(inputs["w_in"][l]), f(inputs["w_out_a"][l]), f(inputs["w_out_r"][l]),
                                 f(inputs["w_out"][l]), f(inputs["w_up"][l]), f(inputs["w_down"][l])) for l in range(depth)])
    pv = np.zeros((128, depth * 128), np.float32)
    for l in range(depth):
        def pc(v):
            return f(v).reshape(8, 128).T
        blk = pv[:, l * 128:(l + 1) * 128]
        blk[:, 0:8] = pc(inputs["norm_pre_mix"][l])
        blk[:, 8:16] = pc(inputs["norm_post_mix"][l])
        blk[:, 16:24] = pc(inputs["norm_pre_ffn"][l])
        blk[:, 24:32] = pc(inputs["norm_post_ffn"][l])
        blk[:, 32:40] = pc(inputs["ret_norm"][l])
        cwv = f(inputs["conv_w"][l])
        cbv = f(inputs["conv_b"][l])
        cw4 = np.concatenate([cwv, cbv[None, :]], axis=0)
        blk[:, 40:128] = cw4.reshape(4, NF, 128).transpose(2, 1, 0).reshape(128, NF * 4)
    sinkb = np.ascontiguousarray(np.broadcast_to(f(inputs["sinks"])[:depth].reshape(1, depth * 8), (128, depth * 8)))
    shared = dict(wall=wall, pvec=pv, sinkb=sinkb, relb=f(inputs["rel_bias"]), oh=c["oh"], decay=c["decay"],
                  xib=c["xib"], zeta=c["zeta"], ident=c["ident"], cosT=c["cosT"], sinT=c["sinT"])
    return shared


def kernel(**inputs):
    x = np.asarray(inputs["x"], dtype=np.float32)
    B = x.shape[0]
    shared = _prep_shared(inputs, DEPTH, SEQ)
    nc = build(SEQ // T, DEPTH)
    in_maps = []
    for b in range(B):
        m = dict(shared)
        m["xT"] = np.ascontiguousarray(x[b].T)
        in_maps.append(m)
    res = run_bass_kernel_spmd(nc, in_maps, core_ids=list(range(B)))
    out = np.stack([np.ascontiguousarray(np.asarray(r["yT"], dtype=np.float32).T) for r in res.results], axis=0)
    return out.astype(np.float32)
```
